# Optimizing a Trainium2 kernel written in Bass

```python
import jax, jax.numpy as jnp
from jax import lax
import numpy as np

D_MODEL = 2048
BATCH = 4
SEQ = 4096
DEPTH = 2

HEAD_DIM = 64
MIX_WIDTH = D_MODEL
A_WIDTH = MIX_WIDTH // 2
A_HEADS = A_WIDTH // HEAD_DIM
A_KV_HEADS = A_HEADS // 4
WINDOW = 128
B_WIDTH = MIX_WIDTH - A_WIDTH
POOL_WINDOWS = (2, 4, 8, 16)
POOL_GROUPS = len(POOL_WINDOWS)
POOL_GW = B_WIDTH // POOL_GROUPS
C_WIDTH = MIX_WIDTH // 2
C_HEADS = 4
C_DV = C_WIDTH // C_HEADS
C_DQK = C_DV // 2
C_CONV = 4
C_CHUNK = 64
FORGET_BIAS = 3.0
D_WIDTH = MIX_WIDTH - C_WIDTH
D_HEADS = D_WIDTH // HEAD_DIM
D_KV_HEADS = D_HEADS // 4
MOBA_BLOCK = 256
MOBA_TOPK = 3
MOBA_QCHUNK = 32
D_FF = 5632
FFN_CONV = 3
NORM_EPS = 1e-6
NEG_INF = -1e30

EVEN_IN = A_WIDTH + 2 * A_KV_HEADS * HEAD_DIM + B_WIDTH
ODD_IN = 2 * C_HEADS * C_DQK + 2 * C_WIDTH + 2 * C_HEADS + D_WIDTH + 2 * D_KV_HEADS * HEAD_DIM
N_EVEN = (DEPTH + 1) // 2
N_ODD = DEPTH // 2

kernel_name = 'hybrid_swa_pool_mlstm_moba_block'


def rms_norm(x, g):
    x32 = x.astype(jnp.float32)
    y = x32 * lax.rsqrt(jnp.mean(x32 * x32, axis=-1, keepdims=True) + NORM_EPS)
    return (y * g.astype(jnp.float32)).astype(x.dtype)


def alibi_slopes(n_heads):
    return jnp.asarray([2.0 ** (-8.0 * (h + 1) / n_heads) for h in range(n_heads)], jnp.float32)


def causal_depthwise_conv(x, w, b):
    K, T = w.shape[0], x.shape[1]
    xp = jnp.pad(x, ((0, 0), (K - 1, 0), (0, 0)))
    y = b
    for j in range(K):
        y = y + xp[:, j:j + T] * w[j]
    return y


def sliding_window_attention(q, k, v, sinks, slopes):
    B, T, H, hd = q.shape
    KV = k.shape[2]
    G = H // KV
    W = WINDOW
    nb = T // W
    qb = q.reshape(B, nb, W, KV, G, hd)

    def band(t):
        tp = jnp.pad(t, ((0, 0), (W, 0), (0, 0), (0, 0))).reshape(B, nb + 1, W, KV, hd)
        return jnp.concatenate([tp[:, :-1], tp[:, 1:]], axis=2)

    kb, vb = band(k), band(v)
    s = jnp.einsum('bnqkgd,bnskd->bnkgqs', qb, kb, preferred_element_type=jnp.float32)
    dist = jnp.arange(W)[:, None] + W - jnp.arange(2 * W)[None, :]
    key_abs = jnp.arange(nb)[:, None] * W + jnp.arange(2 * W)[None, :] - W
    mask = ((dist >= 0) & (dist < W))[None] & (key_abs >= 0)[:, None, :]
    bias = -slopes.reshape(KV, G, 1, 1) * dist.astype(jnp.float32)
    logits = jnp.where(mask[None, :, None, None], s + bias[None, None], NEG_INF)
    sink = jnp.broadcast_to(sinks.astype(jnp.float32).reshape(1, 1, KV, G, 1, 1), logits.shape[:-1] + (1,))
    p = jax.nn.softmax(jnp.concatenate([logits, sink], axis=-1), axis=-1)[..., :-1]
    o = jnp.einsum('bnkgqs,bnskd->bnqkgd', p.astype(v.dtype), vb)
    return o.reshape(B, T, H * hd)


def multiscale_pool_mixer(p, w, b, scale):
    B, T, C = p.shape
    pg = p.reshape(B, T, POOL_GROUPS, POOL_GW).astype(jnp.float32)
    cs = jnp.cumsum(pg, axis=1)
    pos = jnp.arange(1, T + 1, dtype=jnp.float32)
    means = []
    for g, win in enumerate(POOL_WINDOWS):
        c_g = cs[:, :, g]
        lag = jnp.pad(c_g, ((0, 0), (win, 0), (0, 0)))[:, :T]
        means.append((c_g - lag) / jnp.minimum(pos, win)[None, :, None])
    d = (jnp.stack(means, axis=2) - pg).astype(p.dtype)
    y = jnp.einsum('btgc,gcd->btgd', d, w) + b
    return y.reshape(B, T, C) * scale


def mlstm(q, k, v, i_pre, f_pre):
    B, T, H, dqk = q.shape
    dv = v.shape[-1]
    L = C_CHUNK
    nc = T // L
    f32 = jnp.float32

    def chunks(t):
        t = t.astype(f32).reshape((B, nc, L, H) + t.shape[3:])
        return jnp.moveaxis(t, 1, 0).swapaxes(2, 3)

    qs, ks, vs = chunks(q), chunks(k) * (dqk ** -0.5), chunks(v)
    ig = chunks(i_pre)
    lf = chunks(jax.nn.log_sigmoid(f_pre.astype(f32)))
    causal = jnp.tril(jnp.ones((L, L), bool))

    def step(carry, xs):
        C_st, n_st, m = carry
        qc, kc, vc, igc, lfc = xs
        b = jnp.cumsum(lfc, axis=-1)
        logd = jnp.where(causal, b[..., :, None] - b[..., None, :] + igc[..., None, :], -jnp.inf)
        m_inter = m[..., None] + b
        m_row = jnp.maximum(m_inter, jnp.max(logd, axis=-1))
        w_inter = jnp.exp(m_inter - m_row)
        s = jnp.einsum('bhld,bhsd->bhls', qc, kc) * jnp.exp(logd - m_row[..., None])
        num = w_inter[..., None] * jnp.einsum('bhld,bhvd->bhlv', qc, C_st) + jnp.einsum('bhls,bhsv->bhlv', s, vc)
        den = w_inter * jnp.einsum('bhld,bhd->bhl', qc, n_st) + jnp.sum(s, axis=-1)
        h = num / jnp.maximum(jnp.abs(den), jnp.exp(-m_row))[..., None]
        b_last = b[..., -1]
        logw = b_last[..., None] - b + igc
        m_new = jnp.maximum(m + b_last, jnp.max(logw, axis=-1))
        w_old = jnp.exp(m + b_last - m_new)
        ws = jnp.exp(logw - m_new[..., None])
        C_new = w_old[..., None, None] * C_st + jnp.einsum('bhs,bhsv,bhsd->bhvd', ws, vc, kc)
        n_new = w_old[..., None] * n_st + jnp.einsum('bhs,bhsd->bhd', ws, kc)
        return (C_new, n_new, m_new), h

    init = (jnp.zeros((B, H, dv, dqk), f32), jnp.zeros((B, H, dqk), f32), jnp.zeros((B, H), f32))
    _, hs = lax.scan(step, init, (qs, ks, vs, ig, lf))
    return hs.transpose(1, 0, 3, 2, 4).reshape(B, T, H, dv)


def moba_attention(q, k, v, slopes):
    B, T, H, hd = q.shape
    KV = k.shape[2]
    G = H // KV
    BS = MOBA_BLOCK
    QC = MOBA_QCHUNK
    f32 = jnp.float32
    nblk = -(-T // BS)
    Tp = nblk * BS
    pad = ((0, 0), (0, Tp - T), (0, 0), (0, 0))
    q, k, v = jnp.pad(q, pad), jnp.pad(k, pad), jnp.pad(v, pad)
    kb = k.reshape(B, nblk, BS, KV, hd).transpose(0, 3, 1, 2, 4)
    vb = v.reshape(B, nblk, BS, KV, hd).transpose(0, 3, 1, 2, 4)
    k_mean = jnp.mean(kb.astype(f32), axis=3)
    qg = q.reshape(B, Tp, KV, G, hd)
    gate = jnp.einsum('btkgd,bknd->bkgtn', qg.astype(f32), k_mean)
    past = jnp.arange(nblk)[None, :] < (jnp.arange(Tp) // BS)[:, None]
    gate = jnp.where(past, gate, -jnp.inf)
    topk = min(MOBA_TOPK, nblk)
    top_val, top_idx = lax.top_k(gate, topk)
    sel_ok = jnp.isfinite(top_val)
    nqc = Tp // QC
    xs = (qg.reshape(B, nqc, QC, KV, G, hd).transpose(1, 0, 2, 3, 4, 5),
          top_idx.reshape(B, KV, G, nqc, QC, topk).transpose(3, 0, 1, 2, 4, 5),
          sel_ok.reshape(B, KV, G, nqc, QC, topk).transpose(3, 0, 1, 2, 4, 5),
          jnp.arange(nqc) * QC)
    b_ix = jnp.arange(B)[:, None, None, None, None]
    kv_ix = jnp.arange(KV)[None, :, None, None, None]
    offs = jnp.arange(BS)
    slope_sel = slopes.reshape(1, KV, G, 1, 1, 1)
    slope_own = slopes.reshape(1, KV, G, 1, 1)

    def step(args):
        qc, idx, ok, start = args
        qpos = start + jnp.arange(QC)
        kg = kb[b_ix, kv_ix, idx]
        vg = vb[b_ix, kv_ix, idx]
        s_sel = jnp.einsum('bqkgd,bkgqnsd->bkgqns', qc, kg, preferred_element_type=f32)
        pos_sel = idx[..., None] * BS + offs
        s_sel = jnp.where(ok[..., None], s_sel - slope_sel * (qpos[:, None, None] - pos_sel), NEG_INF)
        own = start // BS
        k_own = lax.dynamic_index_in_dim(kb, own, axis=2, keepdims=False)
        v_own = lax.dynamic_index_in_dim(vb, own, axis=2, keepdims=False)
        s_own = jnp.einsum('bqkgd,bksd->bkgqs', qc, k_own, preferred_element_type=f32)
        dist_own = qpos[:, None] - (own * BS + offs)[None, :]
        s_own = jnp.where(dist_own >= 0, s_own - slope_own * dist_own, NEG_INF)
        logits = jnp.concatenate([s_sel.reshape(B, KV, G, QC, topk * BS), s_own], axis=-1)
        p = jax.nn.softmax(logits, axis=-1).astype(v.dtype)
        p_sel = p[..., :topk * BS].reshape(B, KV, G, QC, topk, BS)
        p_own = p[..., topk * BS:]
        return (jnp.einsum('bkgqns,bkgqnsd->bqkgd', p_sel, vg)
                + jnp.einsum('bkgqs,bksd->bqkgd', p_own, v_own))

    out = lax.map(step, xs)
    return out.transpose(1, 0, 2, 3, 4, 5).reshape(B, Tp, H * hd)[:, :T]


def even_mixer(h, w_in, w_out, sinks, pool_w, pool_b, pool_scale, slopes):
    B, T, _ = h.shape
    kvw = A_KV_HEADS * HEAD_DIM
    q, k, v, p = jnp.split(h @ w_in, [A_WIDTH, A_WIDTH + kvw, A_WIDTH + 2 * kvw], axis=-1)
    a = sliding_window_attention(q.reshape(B, T, A_HEADS, HEAD_DIM) * (HEAD_DIM ** -0.5),
                                 k.reshape(B, T, A_KV_HEADS, HEAD_DIM),
                                 v.reshape(B, T, A_KV_HEADS, HEAD_DIM), sinks, slopes)
    bo = multiscale_pool_mixer(p, pool_w, pool_b, pool_scale)
    return jnp.concatenate([a, bo], axis=-1) @ w_out


def odd_mixer(h, w_in, w_out, conv_w, conv_b, gate_b, mh_norm, slopes):
    B, T, _ = h.shape
    kvw = D_KV_HEADS * HEAD_DIM
    sizes = [2 * C_HEADS * C_DQK, C_WIDTH, C_WIDTH, 2 * C_HEADS, D_WIDTH, kvw, kvw]
    qk_c, v_c, o_c, g_c, q_d, k_d, v_d = jnp.split(h @ w_in, np.cumsum(sizes)[:-1].tolist(), axis=-1)
    qk_c = jax.nn.silu(causal_depthwise_conv(qk_c, conv_w, conv_b))
    q_c, k_c = jnp.split(qk_c, 2, axis=-1)
    g_c = g_c + gate_b
    hc = mlstm(q_c.reshape(B, T, C_HEADS, C_DQK), k_c.reshape(B, T, C_HEADS, C_DQK),
               v_c.reshape(B, T, C_HEADS, C_DV), g_c[..., :C_HEADS], g_c[..., C_HEADS:])
    hc = rms_norm(hc, mh_norm.reshape(C_HEADS, C_DV)).astype(h.dtype).reshape(B, T, C_WIDTH)
    hc = hc * jax.nn.sigmoid(o_c)
    hd_out = moba_attention(q_d.reshape(B, T, D_HEADS, HEAD_DIM) * (HEAD_DIM ** -0.5),
                            k_d.reshape(B, T, D_KV_HEADS, HEAD_DIM),
                            v_d.reshape(B, T, D_KV_HEADS, HEAD_DIM), slopes)
    return jnp.concatenate([hc, hd_out], axis=-1) @ w_out


def conv_ffn(h, w_up, conv_w, conv_b, w_down):
    u, g = jnp.split(h @ w_up, 2, axis=-1)
    g = causal_depthwise_conv(g, conv_w, conv_b)
    return (jax.nn.gelu(g, approximate=True) * u) @ w_down


def setup_inputs(seed: int = 0) -> dict:
    key = jax.random.key(seed)
    ks = jax.random.split(key, 25)
    D = D_MODEL

    def nrm(k, shape, s):
        return s * jax.random.normal(k, shape, jnp.float32)

    def gain(k, shape):
        return 1.0 + nrm(k, shape, 0.05)

    return {
        'x': nrm(ks[0], (BATCH, SEQ, D), 1.0),
        'c': nrm(ks[1], (BATCH, D), 1.0),
        'ada_w': nrm(ks[2], (DEPTH, D, 6 * D), D ** -0.5),
        'ada_b': nrm(ks[3], (DEPTH, 6 * D), 0.02),
        'norm_mix_pre': gain(ks[4], (DEPTH, D)),
        'norm_mix_post': gain(ks[5], (DEPTH, D)),
        'norm_ffn_pre': gain(ks[6], (DEPTH, D)),
        'norm_ffn_post': gain(ks[7], (DEPTH, D)),
        'ffn_w_up': nrm(ks[8], (DEPTH, D, 2 * D_FF), D ** -0.5),
        'ffn_conv_w': nrm(ks[9], (DEPTH, FFN_CONV, D_FF), FFN_CONV ** -0.5),
        'ffn_conv_b': nrm(ks[10], (DEPTH, D_FF), 0.02),
        'ffn_w_down': nrm(ks[11], (DEPTH, D_FF, D), D_FF ** -0.5),
        'ev_w_in': nrm(ks[12], (N_EVEN, D, EVEN_IN), D ** -0.5),
        'ev_w_out': nrm(ks[13], (N_EVEN, MIX_WIDTH, D), MIX_WIDTH ** -0.5),
        'ev_sinks': nrm(ks[14], (N_EVEN, A_HEADS), 1.0),
        'ev_pool_w': nrm(ks[15], (N_EVEN, POOL_GROUPS, POOL_GW, POOL_GW), POOL_GW ** -0.5),
        'ev_pool_b': nrm(ks[16], (N_EVEN, POOL_GROUPS, POOL_GW), 0.02),
        'ev_pool_scale': gain(ks[17], (N_EVEN, B_WIDTH)),
        'od_w_in': nrm(ks[18], (N_ODD, D, ODD_IN), D ** -0.5),
        'od_w_out': nrm(ks[19], (N_ODD, MIX_WIDTH, D), MIX_WIDTH ** -0.5),
        'od_conv_w': nrm(ks[20], (N_ODD, C_CONV, 2 * C_HEADS * C_DQK), C_CONV ** -0.5),
        'od_conv_b': nrm(ks[21], (N_ODD, 2 * C_HEADS * C_DQK), 0.02),
        'od_gate_b': jnp.concatenate([nrm(ks[22], (N_ODD, C_HEADS), 0.1),
                                      FORGET_BIAS + nrm(ks[23], (N_ODD, C_HEADS), 0.1)], axis=-1),
        'od_mh_norm': gain(ks[24], (N_ODD, C_WIDTH)),
    }


def reference(x, c, ada_w, ada_b, norm_mix_pre, norm_mix_post, norm_ffn_pre, norm_ffn_post,
              ffn_w_up, ffn_conv_w, ffn_conv_b, ffn_w_down,
              ev_w_in, ev_w_out, ev_sinks, ev_pool_w, ev_pool_b, ev_pool_scale,
              od_w_in, od_w_out, od_conv_w, od_conv_b, od_gate_b, od_mh_norm):
    slopes_a = alibi_slopes(A_HEADS)
    slopes_d = alibi_slopes(D_HEADS)
    c_act = jax.nn.silu(c)
    for layer in range(DEPTH):
        mod = c_act @ ada_w[layer] + ada_b[layer]
        sh_m, sc_m, gt_m, sh_f, sc_f, gt_f = [m[:, None, :] for m in jnp.split(mod, 6, axis=-1)]
        h = rms_norm(x, norm_mix_pre[layer]) * (1.0 + sc_m) + sh_m
        j = layer // 2
        if layer % 2 == 0:
            y = even_mixer(h, ev_w_in[j], ev_w_out[j], ev_sinks[j], ev_pool_w[j], ev_pool_b[j],
                           ev_pool_scale[j], slopes_a)
        else:
            y = odd_mixer(h, od_w_in[j], od_w_out[j], od_conv_w[j], od_conv_b[j], od_gate_b[j],
                          od_mh_norm[j], slopes_d)
        x = x + gt_m * rms_norm(y, norm_mix_post[layer])
        h = rms_norm(x, norm_ffn_pre[layer]) * (1.0 + sc_f) + sh_f
        y = conv_ffn(h, ffn_w_up[layer], ffn_conv_w[layer], ffn_conv_b[layer], ffn_w_down[layer])
        x = x + gt_f * rms_norm(y, norm_ffn_post[layer])
    return x
```

```python
import contextlib
import numpy as np
import ml_dtypes
import concourse.bass as bass
import concourse.mybir as mybir
from concourse.bass_utils import run_bass_kernel_spmd

F32 = mybir.dt.float32
BF16 = mybir.dt.bfloat16
AF = mybir.ActivationFunctionType
ALU = mybir.AluOpType
AX = mybir.AxisListType
NPBF = ml_dtypes.bfloat16

ENGS = ("pe", "act", "dve", "pool", "sp")
SAME_ENGINE_SYNC = True

D = 2048
NTOK = 2048
TT = 512
NTT = NTOK // TT
DFF = 5632
EPS = 1e-6


class Op:
    __slots__ = ("eng", "fn", "deps", "dma", "signal", "sigval", "dsem", "dtarget", "dprev")

    def __init__(self, eng, fn, dma):
        self.eng = eng
        self.fn = fn
        self.dma = dma
        self.deps = []
        self.signal = False
        self.sigval = 0
        self.dsem = None
        self.dtarget = 0
        self.dprev = 0


class Sched:
    def __init__(self, nc, n_dma_sems=8):
        self.nc = nc
        self.ops = {e: [] for e in ENGS}
        self.last_writer = {}
        self.readers = {}
        self.stack = contextlib.ExitStack()
        self.n_dma_sems = n_dma_sems
        self.uid = 0
        self.pstack = None
        self.dma_since = []

    def phase_begin(self):
        assert self.pstack is None
        self.pstack = contextlib.ExitStack()

    def phase_end(self):
        self.pstack.close()
        self.pstack = None
        self.barrier()

    def barrier(self):
        lasts = []
        for e in ENGS:
            for o in reversed(self.ops[e]):
                if not o.dma and o.fn is not None:
                    lasts.append(o)
                    break
        dmas = list(self.dma_since)
        self.dma_since = []
        for e in ENGS:
            o = Op(e, None, False)
            o.deps = list(lasts) + dmas
            self.ops[e].append(o)

    def sbuf(self, name, shape, dtype):
        return (self.pstack if self.pstack is not None else self.stack).enter_context(self.nc.sbuf_tensor("sb_" + name, list(shape), dtype))

    def psum(self, name, shape, dtype=F32):
        return self.stack.enter_context(self.nc.psum_tensor("ps_" + name, list(shape), dtype))

    def sem(self, name):
        return self.stack.enter_context(self.nc.semaphore(name))

    def op(self, eng, fn, reads=(), writes=(), dma=False):
        if eng != "pe":
            bk = [k for k in reads if isinstance(k, tuple) and k and k[0] == "bank"]
            if bk:
                writes = list(writes) + [k for k in bk if k not in writes]
        o = Op(eng, fn, dma)
        deps = {}
        for k in reads:
            w = self.last_writer.get(k)
            if w is not None:
                deps[id(w)] = w
        for k in writes:
            w = self.last_writer.get(k)
            if w is not None:
                deps[id(w)] = w
            for r in self.readers.get(k, ()):
                deps[id(r)] = r
        o.deps = list(deps.values())
        for k in writes:
            self.last_writer[k] = o
            self.readers[k] = []
        for k in reads:
            lst = self.readers.setdefault(k, [])
            if not dma:
                lst[:] = [r for r in lst if r.dma or r.eng != eng]
            lst.append(o)
        self.ops[eng].append(o)
        if dma:
            self.dma_since.append(o)
        return o

    def call(self, eng, meth, *args, reads=(), writes=(), **kw):
        return self.op(eng, lambda e: getattr(e, meth)(*args, **kw), reads, writes)

    def dma(self, eng, out, in_, reads=(), writes=(), **kw):
        return self.op(eng, lambda e: e.dma_start(out=out, in_=in_, **kw), reads, writes, dma=True)

    def mm(self, out, lhsT, rhs, start, stop, reads=(), writes=()):
        return self.op("pe", lambda e: e.matmul(out, lhsT, rhs, start=start, stop=stop), reads, writes)

    def act(self, out, in_, func, reads=(), writes=(), eng="act", **kw):
        return self.op(eng, lambda e: e.activation(out=out, in_=in_, func=func, **kw), reads, writes)

    def emit(self):
        nc = self.nc

        def need_wait(o, d):
            if d.dma:
                return True
            if d.eng == o.eng and not o.dma:
                if o.eng == "pe":
                    return False
                return SAME_ENGINE_SYNC
            return True

        for e in ENGS:
            for o in self.ops[e]:
                o.deps = [d for d in o.deps if need_wait(o, d)]
                for d in o.deps:
                    if not d.dma:
                        d.signal = True
        esem = {}
        for e in ENGS:
            if any((not o.dma) and o.signal for o in self.ops[e]):
                esem[e] = self.sem("done_" + e)
            c = 0
            for o in self.ops[e]:
                if (not o.dma) and o.signal:
                    c += 1
                    o.sigval = c
        for e in ENGS:
            dops = [o for o in self.ops[e] if o.dma]
            if not dops:
                continue
            n = min(self.n_dma_sems, len(dops))
            sems = [self.sem(f"dma_{e}_{i}") for i in range(n)]
            cnt = [0] * n
            for i, o in enumerate(dops):
                s = i % n
                o.dsem = sems[s]
                o.dprev = cnt[s]
                cnt[s] += 16
                o.dtarget = cnt[s]
        all_dma = [o for e in ENGS for o in self.ops[e] if o.dma]

        def emit_engine(e, eng):
            waited = {}

            def wait(sem, val):
                key = id(sem)
                if waited.get(key, 0) >= val:
                    return
                eng.wait_ge(sem, val)
                waited[key] = val

            for o in self.ops[e]:
                for d in o.deps:
                    if d.dma:
                        wait(d.dsem, d.dtarget)
                    else:
                        wait(esem[d.eng], d.sigval)
                if o.fn is None:
                    continue
                if o.dma:
                    if o.dprev > 0:
                        wait(o.dsem, o.dprev)
                    ins = o.fn(eng)
                    ins.then_inc(o.dsem, 16)
                else:
                    ins = o.fn(eng)
                    if o.signal:
                        ins.then_inc(esem[e], 1)
            if e == "sp":
                fin = {}
                for o in all_dma:
                    fin[id(o.dsem)] = (o.dsem, max(o.dtarget, fin.get(id(o.dsem), (None, 0))[1]))
                for sem_, v_ in fin.values():
                    wait(sem_, v_)

        with nc.Block() as block:
            @block.tensor
            def _(eng):
                emit_engine("pe", eng)

            @block.scalar
            def _(eng):
                emit_engine("act", eng)

            @block.vector
            def _(eng):
                emit_engine("dve", eng)

            @block.gpsimd
            def _(eng):
                emit_engine("pool", eng)

            @block.sync
            def _(eng):
                emit_engine("sp", eng)

    def close(self):
        self.stack.close()


class Rot:
    def __init__(self, S, name, n, shape, dtype, psum=False):
        self.tiles = [(S.psum if psum else S.sbuf)(f"{name}{i}", shape, dtype) for i in range(n)]
        self.keys = [(name, i) for i in range(n)]
        self.i = -1

    def next(self):
        self.i = (self.i + 1) % len(self.tiles)
        return self.tiles[self.i], self.keys[self.i]


class BankRot:
    def __init__(self, tiles, keys):
        self.tiles, self.keys, self.i = tiles, keys, -1

    def next(self):
        self.i = (self.i + 1) % len(self.tiles)
        return self.tiles[self.i], self.keys[self.i]


class Banks:
    def __init__(self, S):
        self.t = [S.psum(f"bank{i}", [128, 512], F32) for i in range(8)]

    def rot(self, idxs):
        return BankRot([self.t[i] for i in idxs], [("bank", i) for i in idxs])


def build_L0():
    nc = bass.Bass("TRN2", target_bir_lowering=False)
    S = Sched(nc)
    NCOL = 3072
    cT_d = nc.dram_tensor("cT", [D, 4], F32, kind="ExternalInput").ap()
    w_d = nc.dram_tensor("w", [D, NCOL], F32, kind="ExternalInput").ap()
    b_d = nc.dram_tensor("b", [128, NCOL // 128], F32, kind="ExternalInput").ap()
    o_d = nc.dram_tensor("mod", [128, NCOL // 128 * 4], F32, kind="ExternalOutput").ap()
    c_sb = S.sbuf("c_sb", [128, 16, 4], F32)
    ca = S.sbuf("ca", [128, 16, 4], F32)
    b_sb = S.sbuf("b_sb", [128, 24], F32)
    m_sb = S.sbuf("m_sb", [128, 24, 4], F32)
    ps = S.psum("ps", [128, 24, 4])
    wr = Rot(S, "w", 2, [128, 16, 512], F32)
    S.dma("sp", c_sb[:], cT_d.rearrange("(c p) n -> p c n", p=128), writes=["c"])
    S.dma("sp", b_sb[:], b_d, writes=["b"])
    S.act(ca[:], c_sb[:], AF.Silu, reads=["c"], writes=["ca"])
    wv = w_d.rearrange("(c p) n -> p c n", p=128)
    for g in range(6):
        wt, wk = wr.next()
        S.dma("sp", wt[:], wv[:, :, g * 512:(g + 1) * 512], writes=[wk])
        for j in range(4):
            for c in range(16):
                S.mm(ps[:, g * 4 + j, :], wt[:, c, j * 128:(j + 1) * 128], ca[:, c, :], c == 0, c == 15,
                     reads=[wk, "ca"], writes=["ps"])
    S.call("dve", "tensor_tensor", out=m_sb[:], in0=ps[:], in1=b_sb[:].unsqueeze(2).to_broadcast([128, 24, 4]),
           op=ALU.add, reads=["ps", "b"], writes=["m"])
    S.dma("sp", o_d, m_sb[:].rearrange("p a b -> p (a b)"), reads=["m"])
    S.emit()
    S.close()
    return nc


def run_L0(inp):
    W = np.concatenate([inp["ada_w"][0], inp["ada_w"][1]], axis=1)
    Bv = np.concatenate([inp["ada_b"][0], inp["ada_b"][1]], axis=0)
    cT = np.ascontiguousarray(inp["c"].T)
    maps = []
    for i in range(8):
        maps.append({"cT": cT, "w": np.ascontiguousarray(W[:, i * 3072:(i + 1) * 3072]),
                     "b": np.ascontiguousarray(Bv[i * 3072:(i + 1) * 3072].reshape(24, 128).T)})
    res = run_bass_kernel_spmd(build_L0(), maps, core_ids=list(range(8)))
    mod = np.zeros((4, 24576), np.float32)
    for i in range(8):
        m = res.results[i]["mod"].reshape(128, 24, 4)
        mod[:, i * 3072:(i + 1) * 3072] = m.transpose(2, 1, 0).reshape(4, 3072)
    return [mod[:, :12288], mod[:, 12288:]]


def vec_pc(v):
    return np.ascontiguousarray(np.asarray(v, np.float32).reshape(-1, 128).T)


class Common:
    def __init__(self, S, nxt=2):
        self.S = S
        self.ones = S.sbuf("ones_bf", [128, 128], BF16)
        S.call("dve", "memset", self.ones[:], 1.0, writes=["ones"])
        self.eps = S.sbuf("eps_c", [128, 1], F32)
        S.call("dve", "memset", self.eps[:], EPS, writes=["epsc"])
        self.banks = Banks(S)
        self.ps_ssq = self.banks.rot([7])
        self.xt = Rot(S, "xt", nxt, [128, 16, TT], F32) if nxt else None
        self.sq = Rot(S, "sq", 1, [128, 16, TT], BF16)
        self.rstd = Rot(S, "rstd", 2, [128, TT], F32)


def emit_rstd(S, C, src, src_key, nchunks=16, extra_reads=()):
    sq, sqk = C.sq.next()
    S.act(sq[:, 0:nchunks, :], src, AF.Square, reads=([src_key] if src_key is not None else []) + list(extra_reads), writes=[sqk])
    ps, psk = C.ps_ssq.next()
    for c in range(nchunks):
        S.mm(ps[:], C.ones[:], sq[:, c, :], c == 0, c == nchunks - 1, reads=[sqk, "ones"], writes=[psk])
    r, rk = C.rstd.next()
    S.act(r[:], ps[:], AF.Sqrt, scale=1.0 / D, bias=C.eps[:], reads=[psk, "epsc"], writes=[rk])
    S.call("dve", "reciprocal", out=r[:], in_=r[:], reads=[rk], writes=[rk])
    return r, rk


def emit_norm_hT(S, C, xT_d, gm, sh, vk, hT, ntok=NTOK, tok0=0, hkey="hT"):
    xv = xT_d.rearrange("(c p) t -> p c t", p=128)
    for t in range(ntok // TT):
        xt, xk = C.xt.next()
        S.dma("sp", xt[:], xv[:, :, tok0 + t * TT: tok0 + (t + 1) * TT], writes=[xk])
        r, rk = emit_rstd(S, C, xt[:], xk)
        S.call("dve", "tensor_tensor", out=xt[:], in0=xt[:], in1=r[:].unsqueeze(1).to_broadcast([128, 16, TT]),
               op=ALU.mult, reads=[xk, rk], writes=[xk])
        for c in range(16):
            S.act(hT[:, c, t * TT:(t + 1) * TT], xt[:, c, :], AF.Identity, scale=gm[:, c:c + 1], bias=sh[:, c:c + 1],
                  reads=[xk] + list(vk), writes=[(hkey, t)])


def emit_modvecs(S, modp_d, g1_d, g2_d, g3_d, g4_d):
    S_ = S
    modp = S_.sbuf("modp_sb", [128, 96], F32)
    gs = S_.sbuf("gains_sb", [128, 64], F32)
    vec = S_.sbuf("vecs_sb", [128, 64], F32)
    S_.dma("sp", modp[:], modp_d, writes=["modp"])
    for i, gd in enumerate((g1_d, g2_d, g3_d, g4_d)):
        S_.dma("sp", gs[:, i * 16:(i + 1) * 16], gd, writes=[("gains", i)])
    S_.call("dve", "scalar_tensor_tensor", out=vec[:, 0:16], in0=modp[:, 16:32], scalar=1.0, in1=gs[:, 0:16],
            op0=ALU.add, op1=ALU.mult, reads=["modp", ("gains", 0)], writes=["vecs0"])
    S_.call("dve", "tensor_tensor", out=vec[:, 16:32], in0=modp[:, 32:48], in1=gs[:, 16:32], op=ALU.mult,
            reads=["modp", ("gains", 1)], writes=["vecs1"])
    S_.call("dve", "scalar_tensor_tensor", out=vec[:, 32:48], in0=modp[:, 64:80], scalar=1.0, in1=gs[:, 32:48],
            op0=ALU.add, op1=ALU.mult, reads=["modp", ("gains", 2)], writes=["vecs2"])
    S_.call("dve", "tensor_tensor", out=vec[:, 48:64], in0=modp[:, 80:96], in1=gs[:, 48:64], op=ALU.mult,
            reads=["modp", ("gains", 3)], writes=["vecs3"])
    return dict(gm_m=vec[:, 0:16], sh_m=modp[:, 0:16], gg_m=vec[:, 16:32],
                gm_f=vec[:, 32:48], sh_f=modp[:, 48:64], gg_f=vec[:, 48:64],
                k_m="vecs0", k_gm="vecs1", k_f="vecs2", k_gf="vecs3")


def _keys(k):
    return list(k) if isinstance(k, (list, tuple)) and not (len(k) == 2 and isinstance(k[0], str) and isinstance(k[1], int)) else [k]


def decl_vec_inputs(nc):
    modp_d = nc.dram_tensor("modp", [128, 96], F32, kind="ExternalInput").ap()
    gds = [nc.dram_tensor(f"gain{i}", [128, 16], F32, kind="ExternalInput").ap() for i in range(4)]
    return modp_d, gds


def emit_proj_fm(S, hT, wt, wk, col0, M, psr, out_tile, out_key, evac_eng, scale=1.0, hkey="hT", ntt=NTT):
    for t in range(ntt):
        ps, pk = psr.next()
        for c in range(16):
            S.mm(ps[0:M, :], wt[:, c, col0:col0 + M], hT[:, c, t * TT:(t + 1) * TT], c == 0, c == 15,
                 reads=[wk, (hkey, t)], writes=[pk])
        if evac_eng == "act":
            S.act(out_tile[0:M, t * TT:(t + 1) * TT], ps[0:M, :], AF.Copy, scale=scale, reads=[pk], writes=[out_key])
        else:
            S.call("dve", "tensor_copy", out=out_tile[0:M, t * TT:(t + 1) * TT], in_=ps[0:M, :], reads=[pk], writes=[out_key])


def build_L1():
    nc = bass.Bass("TRN2", target_bir_lowering=False)
    S = Sched(nc)
    xT_d = nc.dram_tensor("xT", [D, NTOK], F32, kind="ExternalInput").ap()
    modp_d, gds = decl_vec_inputs(nc)
    w_d = nc.dram_tensor("w_in", [D, 2560], F32, kind="ExternalInput").ap()
    qT_d = nc.dram_tensor("qT", [16, 64, NTOK], BF16, kind="ExternalOutput").ap()
    kT_d = nc.dram_tensor("kT", [4, 64, NTOK], BF16, kind="ExternalOutput").ap()
    v_d = nc.dram_tensor("v", [NTOK, 256], BF16, kind="ExternalOutput").ap()
    pT_d = nc.dram_tensor("pT", [1024, NTOK], F32, kind="ExternalOutput").ap()
    C = Common(S)
    V = emit_modvecs(S, modp_d, *gds)
    hT = S.sbuf("hT_sb", [128, 16, NTOK], BF16)
    emit_norm_hT(S, C, xT_d, V["gm_m"], V["sh_m"], [V["k_m"], "modp"], hT)
    wv = w_d.rearrange("(c p) n -> p c n", p=128)
    wr = Rot(S, "wbuf", 2, [128, 16, 512], BF16)
    psr = C.banks.rot([0, 1, 2, 3])
    qst = Rot(S, "qst", 2, [64, NTOK], BF16)
    pst = Rot(S, "pst", 1, [128, NTOK], F32)
    vst = S.sbuf("vst_sb", [128, 16, 256], BF16)
    for g in range(2):
        wt, wk = wr.next()
        S.dma("pool", wt[:], wv[:, :, g * 512:(g + 1) * 512], writes=[wk])
        for hh in range(8):
            st, sk = qst.next()
            emit_proj_fm(S, hT, wt, wk, hh * 64, 64, psr, st, sk, "act", scale=0.125)
            S.dma("sp", qT_d[g * 8 + hh], st[:], reads=[sk])
    wt, wk = wr.next()
    S.dma("pool", wt[:], wv[:, :, 1024:1536], writes=[wk])
    for j in range(4):
        st, sk = qst.next()
        emit_proj_fm(S, hT, wt, wk, j * 64, 64, psr, st, sk, "act")
        S.dma("sp", kT_d[j], st[:], reads=[sk])
    for tt in range(16):
        ps, pk = psr.next()
        for c in range(16):
            S.mm(ps[:, 0:256], hT[:, c, tt * 128:(tt + 1) * 128], wt[:, c, 256:512], c == 0, c == 15,
                 reads=[wk, ("hT", tt // 4)], writes=[pk])
        S.call("dve", "tensor_copy", out=vst[:, tt, :], in_=ps[:, 0:256], reads=[pk], writes=["vst"])
    S.dma("sp", v_d.rearrange("(t p) n -> p t n", p=128), vst[:], reads=["vst"])
    for g in range(2):
        wt, wk = wr.next()
        S.dma("pool", wt[:], wv[:, :, 1536 + g * 512:1536 + (g + 1) * 512], writes=[wk])
        for j in range(4):
            st, sk = pst.next()
            emit_proj_fm(S, hT, wt, wk, j * 128, 128, psr, st, sk, "dve")
            S.dma("sp", pT_d[(g * 4 + j) * 128:(g * 4 + j + 1) * 128, :], st[:], reads=[sk])
    S.emit()
    S.close()
    return nc


def modp_of(mod_row):
    return np.ascontiguousarray(mod_row.reshape(96, 128).T)


def core_vec_inputs(inp, mods, layer, b):
    d = {"modp": modp_of(mods[layer][b])}
    for i, nm in enumerate(("norm_mix_pre", "norm_mix_post", "norm_ffn_pre", "norm_ffn_post")):
        d[f"gain{i}"] = vec_pc(inp[nm][layer])
    return d


def swa_consts():
    slopes = np.array([2.0 ** (-8.0 * (h + 1) / 16) for h in range(16)], np.float64)
    s = np.arange(128)[:, None, None]
    q = np.arange(128)[None, None, :]
    sl = slopes[None, :, None]
    own = np.where(q >= s, np.exp(-sl * (q - s)), 0.0)
    prev = np.where(s > q, np.exp(-sl * (q + 128 - s)), 0.0)
    return np.ascontiguousarray(np.stack([own, prev], axis=1).astype(np.float32))


def emit_swa(S, B, qT_d, kTe_d, ve_d, mask_d, flag_d, sinkbc_d, ident_d, mixT_d):
    q_r = Rot(S, "q_sb", 2, [64, 16, TT], BF16)
    k_sb = S.sbuf("k_sb", [64, 4, NTOK + 128], BF16)
    v_sb = S.sbuf("v_sb", [128, 17, 4, 65], BF16)
    msk = S.sbuf("msk", [128, 2, 16, 128], F32)
    mp0 = S.sbuf("mp0", [128, 16, 128], F32)
    flag = S.sbuf("flag", [128, 1], F32)
    esink = S.sbuf("esink", [128, 16], F32)
    ident = S.sbuf("ident", [128, 128], BF16)
    S.dma("sp", k_sb[:], kTe_d.rearrange("h d t -> d h t"), writes=["k_sb"])
    S.call("dve", "memset", v_sb[:], 1.0, writes=["v_sb"])
    for j in range(4):
        S.dma("sp", v_sb[:, :, j, 0:64], ve_d.rearrange("(c p) n -> p c n", p=128)[:, :, j * 64:(j + 1) * 64],
              writes=["v_sb"], reads=["v_sb"])
    S.dma("sp", msk[:], mask_d, writes=["msk"])
    S.dma("sp", flag[:], flag_d, writes=["flag"])
    S.dma("sp", esink[:], sinkbc_d, writes=["esink"])
    S.dma("sp", ident[:], ident_d, writes=["ident"])
    S.act(esink[:], esink[:], AF.Exp, reads=["esink"], writes=["esink"])
    S.call("dve", "tensor_scalar", out=mp0[:], in0=msk[:, 1, :, :], scalar1=flag[:, 0:1], scalar2=None, op0=ALU.mult,
           reads=["msk", "flag"], writes=["mp0"])
    ps_s = B.rot([0, 1])
    ps_o = B.rot([2, 3])
    ps_t = B.rot([4])
    ef = Rot(S, "ef", 2, [128, 512], F32)
    eT = Rot(S, "eT", 4, [128, 512], BF16)
    atok = Rot(S, "atok", 2, [128, 16, 64], BF16)
    ast = Rot(S, "ast", 2, [128, 8, TT], BF16)
    den = Rot(S, "den", 2, [128, 4], F32)
    for n in range(16):
        at, ak = atok.next()
        if n % 4 == 0:
            st, stk = ast.next()
            q_sb, qk = q_r.next()
            S.dma("sp", q_sb[:], qT_d.rearrange("h d t -> d h t")[:, :, (n // 4) * TT:(n // 4 + 1) * TT], writes=[qk])
        for j in range(4):
            ets = []
            for ci in range(2):
                ch = n + 1 - ci
                ps, pk = ps_s.next()
                S.mm(ps[:].rearrange("p (h q) -> p h q", h=4), k_sb[:, j, ch * 128:(ch + 1) * 128],
                     q_sb[:, 4 * j:4 * j + 4, (n % 4) * 128:(n % 4 + 1) * 128], True, True, reads=["k_sb", qk], writes=[pk])
                e, ek = ef.next()
                S.act(e[:], ps[:], AF.Exp, reads=[pk], writes=[ek])
                et, etk = eT.next()
                if ci == 0:
                    m, mk = msk[:, 0, 4 * j:4 * j + 4, :], "msk"
                elif n == 0:
                    m, mk = mp0[:, 4 * j:4 * j + 4, :], "mp0"
                else:
                    m, mk = msk[:, 1, 4 * j:4 * j + 4, :], "msk"
                S.call("dve", "tensor_tensor", out=et[:].rearrange("p (h q) -> p h q", h=4),
                       in0=e[:].rearrange("p (h q) -> p h q", h=4), in1=m, op=ALU.mult, reads=[ek, mk], writes=[etk])
                ets.append((et, etk, ch))
            po, pok = ps_o.next()
            pov = po[:, 0:260].rearrange("p (h d) -> p h d", d=65)
            first = True
            for h4 in range(4):
                for ci, (et, etk, ch) in enumerate(ets):
                    S.mm(pov[:, h4, :], et[:, h4 * 128:(h4 + 1) * 128], v_sb[:, ch, j, :], first, (h4 == 3 and ci == 1),
                         reads=[etk, "v_sb"], writes=[pok])
                    first = False
            dn, dk = den.next()
            S.call("dve", "tensor_tensor", out=dn[:], in0=pov[:, :, 64], in1=esink[:, 4 * j:4 * j + 4], op=ALU.add,
                   reads=[pok, "esink"], writes=[dk])
            S.call("dve", "reciprocal", out=dn[:], in_=dn[:], reads=[dk], writes=[dk])
            S.call("dve", "tensor_tensor", out=at[:, 4 * j:4 * j + 4, :], in0=pov[:, :, 0:64],
                   in1=dn[:].unsqueeze(2).to_broadcast([128, 4, 64]), op=ALU.mult, reads=[pok, dk], writes=[ak])
        pt, ptk = ps_t.next()
        ptv = pt[:].bitcast(BF16).rearrange("p (c t) -> p c t", c=8)
        atv = at[:].rearrange("p h d -> p (h d)")
        for cc in range(8):
            S.op("pe", (lambda e, o=ptv[:, cc, :], i=atv[:, cc * 128:(cc + 1) * 128]: e.transpose(o, i, ident[:])),
                 reads=[ak, "ident"], writes=[ptk])
        nb = n % 4
        S.call("act", "copy", out=st[:, :, nb * 128:(nb + 1) * 128], in_=ptv, reads=[ptk], writes=[stk])
        if nb == 3:
            t = n // 4
            S.dma("sp", mixT_d[0:1024, t * TT:(t + 1) * TT].rearrange("(c p) t -> p c t", p=128), st[:], reads=[stk])


def emit_pool(S, B, pTe_d, invpos_d, pw_d, pb_d, pscale_d, mixT_d):
    pw = S.sbuf("pw_sb", [128, 4, 2, 256], BF16)
    pb = S.sbuf("pb_sb", [128, 8], F32)
    psc = S.sbuf("psc_sb", [128, 8], F32)
    inv = S.sbuf("inv_sb", [128, 4, TT], F32)
    S.dma("pool", pw[:], pw_d.rearrange("g (cc p) d -> p g cc d", p=128), writes=["pw"])
    S.dma("sp", pb[:], pb_d, writes=["pb"])
    S.dma("sp", psc[:], pscale_d, writes=["psc"])
    S.dma("sp", inv[:], invpos_d, writes=["inv"])
    S.call("dve", "tensor_tensor", out=pb[:], in0=pb[:], in1=psc[:], op=ALU.mult, reads=["pb", "psc"], writes=["pb"])
    pv = pTe_d.rearrange("(c p) t -> p c t", p=128)
    p_r = Rot(S, "p_sb", 2, [128, 8, TT + 16], F32)
    ta = S.sbuf("pool_ta", [128, 2, TT + 16], F32)
    tb = S.sbuf("pool_tb", [128, 2, TT + 16], F32)
    d_sb = S.sbuf("pool_d", [128, 8, TT], BF16)
    bo = Rot(S, "bo_st", 2, [128, 8, TT], BF16)
    ps_p = B.rot([5, 6])
    W = TT + 16
    for t in range(NTT):
        p, pk = p_r.next()
        S.dma("sp", p[:], pv[:, :, t * TT:t * TT + W], writes=[pk])
        for g in range(4):
            P = p[:, 2 * g:2 * g + 2, :]
            S.call("dve", "tensor_tensor", out=ta[:, :, 1:W], in0=P[:, :, 1:W], in1=P[:, :, 0:W - 1], op=ALU.add,
                   reads=[pk], writes=["ta"])
            cur, ck, oth, ok = ta, "ta", tb, "tb"
            lag = 2
            for _ in range(g):
                lo = 2 * lag - 1
                S.call("dve", "tensor_tensor", out=oth[:, :, lo:W], in0=cur[:, :, lo:W], in1=cur[:, :, lo - lag:W - lag],
                       op=ALU.add, reads=[ck], writes=[ok])
                cur, ck, oth, ok = oth, ok, cur, ck
                lag *= 2
            win = 2 ** (g + 1)
            dst = d_sb[:, 2 * g:2 * g + 2, :]
            if t == 0:
                S.call("dve", "tensor_tensor", out=cur[:, :, 16:W], in0=cur[:, :, 16:W],
                       in1=inv[:, g, :].unsqueeze(1).to_broadcast([128, 2, TT]), op=ALU.mult, reads=[ck, "inv"], writes=[ck])
                S.call("dve", "tensor_tensor", out=dst, in0=cur[:, :, 16:W], in1=P[:, :, 16:W], op=ALU.subtract,
                       reads=[ck, pk], writes=[("d", g)])
            else:
                S.call("dve", "scalar_tensor_tensor", out=dst, in0=cur[:, :, 16:W], scalar=1.0 / win, in1=P[:, :, 16:W],
                       op0=ALU.mult, op1=ALU.subtract, reads=[ck, pk], writes=[("d", g)])
        b_, bk = bo.next()
        for g in range(4):
            for dt in range(2):
                ps, psk = ps_p.next()
                for cc in range(2):
                    S.mm(ps[:], pw[:, g, cc, dt * 128:(dt + 1) * 128], d_sb[:, 2 * g + cc, :], cc == 0, cc == 1,
                         reads=["pw", ("d", g)], writes=[psk])
                S.act(b_[:, 2 * g + dt, :], ps[:], AF.Identity, scale=psc[:, 2 * g + dt:2 * g + dt + 1],
                      bias=pb[:, 2 * g + dt:2 * g + dt + 1], reads=[psk, "pb", "psc"], writes=[bk])
        S.dma("sp", mixT_d[1024:2048, t * TT:(t + 1) * TT].rearrange("(c p) t -> p c t", p=128), b_[:], reads=[bk])


def build_L2a():
    nc = bass.Bass("TRN2", target_bir_lowering=False)
    S = Sched(nc)
    qT_d = nc.dram_tensor("qT", [16, 64, NTOK], BF16, kind="ExternalInput").ap()
    kTe_d = nc.dram_tensor("kTe", [4, 64, NTOK + 128], BF16, kind="ExternalInput").ap()
    ve_d = nc.dram_tensor("ve", [NTOK + 128, 256], BF16, kind="ExternalInput").ap()
    mask_d = nc.dram_tensor("swa_mask", [128, 2, 16, 128], F32, kind="ExternalInput").ap()
    flag_d = nc.dram_tensor("flag", [128, 1], F32, kind="ExternalInput").ap()
    sink_d = nc.dram_tensor("sinkbc", [128, 16], F32, kind="ExternalInput").ap()
    ident_d = nc.dram_tensor("ident", [128, 128], BF16, kind="ExternalInput").ap()
    pTe_d = nc.dram_tensor("pTe", [1024, NTOK + 16], F32, kind="ExternalInput").ap()
    inv_d = nc.dram_tensor("invpos", [128, 4, TT], F32, kind="ExternalInput").ap()
    pw_d = nc.dram_tensor("pool_w", [4, 256, 256], F32, kind="ExternalInput").ap()
    pb_d = nc.dram_tensor("pool_b", [128, 8], F32, kind="ExternalInput").ap()
    psc_d = nc.dram_tensor("pool_scale", [128, 8], F32, kind="ExternalInput").ap()
    mixT_d = nc.dram_tensor("mixT", [D, NTOK], BF16, kind="ExternalOutput").ap()
    B = Banks(S)
    emit_swa(S, B, qT_d, kTe_d, ve_d, mask_d, flag_d, sink_d, ident_d, mixT_d)
    emit_pool(S, B, pTe_d, inv_d, pw_d, pb_d, psc_d, mixT_d)
    S.emit()
    S.close()
    return nc


def invpos_table(s):
    pos = np.arange(1, TT + 1, dtype=np.float64) + s * NTOK
    t = np.stack([1.0 / np.minimum(pos, w) for w in (2, 4, 8, 16)], axis=0)
    return np.ascontiguousarray(np.broadcast_to(t[None], (128, 4, TT)).astype(np.float32))


def halo_cat(prev, cur, n, axis):
    if prev is None:
        shp = list(cur.shape)
        shp[axis] = n
        h = np.zeros(shp, cur.dtype)
    else:
        h = np.take(prev, range(prev.shape[axis] - n, prev.shape[axis]), axis=axis)
    return np.ascontiguousarray(np.concatenate([h, cur], axis=axis))


def emit_G2(S, C, actT_d, KC, W_d, xin_d, xout_d, gg, ggk, tag, nabuf=2):
    av = actT_d.rearrange("(c p) t -> p c t", p=128)
    wv = W_d.rearrange("(c p) n -> p c n", p=128)
    xiv = xin_d.rearrange("(c p) t -> p c t", p=128)
    xov = xout_d.rearrange("(c p) t -> p c t", p=128)
    a_r = Rot(S, tag + "a", nabuf, [128, KC, TT], BF16)
    w_r = Rot(S, tag + "w", 2, [128, KC, 256], BF16)
    yT = S.sbuf(tag + "yT", [128, 16, TT], F32)
    xc = Rot(S, tag + "xc", 4, [128, TT], F32)
    psr = C.banks.rot([0, 1, 2, 3])
    yk = tag + "yT"
    for t in range(NTT):
        a, ak = a_r.next()
        S.dma("sp", a[:], av[:, :, t * TT:(t + 1) * TT], writes=[ak])
        for g in range(8):
            w, wk = w_r.next()
            S.dma("pool", w[:], wv[:, :, g * 256:(g + 1) * 256], writes=[wk])
            for j in range(2):
                n = g * 2 + j
                ps, pk = psr.next()
                for kc in range(KC):
                    S.mm(ps[:], w[:, kc, j * 128:(j + 1) * 128], a[:, kc, :], kc == 0, kc == KC - 1,
                         reads=[wk, ak], writes=[pk])
                S.call("act", "copy", out=yT[:, n, :], in_=ps[:], reads=[pk], writes=[(yk, n)])
        r, rk = emit_rstd(S, C, yT[:], None, extra_reads=[(yk, n) for n in range(16)])
        S.call("dve", "tensor_tensor", out=yT[:], in0=yT[:], in1=r[:].unsqueeze(1).to_broadcast([128, 16, TT]),
               op=ALU.mult, reads=[rk] + [(yk, n) for n in range(16)], writes=[(yk, n) for n in range(16)])
        for n in range(16):
            x, xk = xc.next()
            S.dma("sp", x[:], xiv[:, n, t * TT:(t + 1) * TT], writes=[xk])
            S.call("dve", "scalar_tensor_tensor", out=x[:], in0=yT[:, n, :], scalar=gg[:, n:n + 1], in1=x[:],
                   op0=ALU.mult, op1=ALU.add, reads=[(yk, n), xk, ggk], writes=[xk])
            S.dma("sp", xov[:, n, t * TT:(t + 1) * TT], x[:], reads=[xk])


def build_L2b():
    nc = bass.Bass("TRN2", target_bir_lowering=False)
    S = Sched(nc)
    mixT_d = nc.dram_tensor("mixT", [D, NTOK], BF16, kind="ExternalInput").ap()
    w_d = nc.dram_tensor("w_out", [D, D], F32, kind="ExternalInput").ap()
    xT_d = nc.dram_tensor("xT", [D, NTOK], F32, kind="ExternalInput").ap()
    modp_d, gds = decl_vec_inputs(nc)
    xo_d = nc.dram_tensor("xoT", [D, NTOK], F32, kind="ExternalOutput").ap()
    C = Common(S, nxt=0)
    V = emit_modvecs(S, modp_d, *gds)
    emit_G2(S, C, mixT_d, 16, w_d, xT_d, xo_d, V["gg_m"], V["k_gm"], "g2")
    S.emit()
    S.close()
    return nc


def emit_ffn_up(S, C, xT_d, xh_d, flag_d, gm, sh, vkeys, wup_d, cw_d, cb_d, aT_d):
    hT = S.sbuf("f_hT", [128, 16, NTOK], BF16)
    hh = S.sbuf("f_hh", [128, 16, 2], BF16)
    xh = S.sbuf("f_xh", [128, 16, 2], F32)
    sqh = S.sbuf("f_sqh", [128, 16, 2], BF16)
    rh = S.sbuf("f_rh", [128, 2], F32)
    flag = S.sbuf("f_flag", [128, 1], F32)
    cw = S.sbuf("f_cw", [128, 44, 3], F32)
    cb = S.sbuf("f_cb", [128, 44], F32)
    S.dma("sp", flag[:], flag_d, writes=["f_flag"])
    S.dma("sp", cw[:], cw_d, writes=["f_cw"])
    S.dma("sp", cb[:], cb_d, writes=["f_cb"])
    emit_norm_hT(S, C, xT_d, gm, sh, vkeys, hT, hkey="f_hT")
    S.dma("sp", xh[:], xh_d.rearrange("(c p) t -> p c t", p=128), writes=["f_xh"])
    S.act(sqh[:], xh[:], AF.Square, reads=["f_xh"], writes=["f_sqh"])
    ps, pk = C.ps_ssq.next()
    for c in range(16):
        S.mm(ps[:, 0:2], C.ones[:], sqh[:, c, :], c == 0, c == 15, reads=["f_sqh", "ones"], writes=[pk])
    S.act(rh[:], ps[:, 0:2], AF.Sqrt, scale=1.0 / D, bias=C.eps[:], reads=[pk, "epsc"], writes=["f_rh"])
    S.call("dve", "reciprocal", out=rh[:], in_=rh[:], reads=["f_rh"], writes=["f_rh"])
    S.call("dve", "tensor_tensor", out=xh[:], in0=xh[:], in1=rh[:].unsqueeze(1).to_broadcast([128, 16, 2]), op=ALU.mult,
           reads=["f_xh", "f_rh"], writes=["f_xh"])
    for c in range(16):
        S.act(hh[:, c, :], xh[:, c, :], AF.Identity, scale=gm[:, c:c + 1], bias=sh[:, c:c + 1],
              reads=["f_xh"] + list(vkeys), writes=["f_hh"])
    wv = wup_d.rearrange("(c p) n -> p c n", p=128)
    wu_r = Rot(S, "f_wu", 2, [128, 16, 256], BF16)
    wg_r = Rot(S, "f_wg", 2, [128, 16, 256], BF16)
    gb_r = Rot(S, "f_gb", 2, [128, NTOK + 2], F32)
    tm_r = Rot(S, "f_tm", 2, [128, TT], F32)
    ge_r = Rot(S, "f_ge", 2, [128, TT], F32)
    as_r = Rot(S, "f_as", 2, [128, NTOK], BF16)
    ps_u = C.banks.rot([0, 1])
    ps_g = C.banks.rot([2, 3])
    ps_h = C.banks.rot([4])
    for i in range(44):
        if i % 2 == 0:
            wu, wuk = wu_r.next()
            wg, wgk = wg_r.next()
            S.dma("pool", wu[:], wv[:, :, i * 128:i * 128 + 256], writes=[wuk])
            S.dma("pool", wg[:], wv[:, :, DFF + i * 128:DFF + i * 128 + 256], writes=[wgk])
        co = (i % 2) * 128
        gb, gbk = gb_r.next()
        ast, ask = as_r.next()
        ph, phk = ps_h.next()
        for c in range(16):
            S.mm(ph[:, 0:2], wg[:, c, co:co + 128], hh[:, c, :], c == 0, c == 15, reads=[wgk, "f_hh"], writes=[phk])
        S.act(gb[:, 0:2], ph[:, 0:2], AF.Copy, scale=flag[:, 0:1], reads=[phk, "f_flag"], writes=[(gbk, -1)])
        for t in range(NTT):
            pu, puk = ps_u.next()
            pg, pgk = ps_g.next()
            for c in range(16):
                S.mm(pg[:], wg[:, c, co:co + 128], hT[:, c, t * TT:(t + 1) * TT], c == 0, c == 15,
                     reads=[wgk, ("f_hT", t)], writes=[pgk])
            for c in range(16):
                S.mm(pu[:], wu[:, c, co:co + 128], hT[:, c, t * TT:(t + 1) * TT], c == 0, c == 15,
                     reads=[wuk, ("f_hT", t)], writes=[puk])
            o = 2 + t * TT
            S.call("act", "copy", out=gb[:, o:o + TT], in_=pg[:], reads=[pgk], writes=[(gbk, t)])
            tm, tmk = tm_r.next()
            S.call("dve", "tensor_scalar", out=tm[:], in0=gb[:, o:o + TT], scalar1=cw[:, i, 2:3], scalar2=cb[:, i:i + 1],
                   op0=ALU.mult, op1=ALU.add, reads=[(gbk, t), "f_cw", "f_cb"], writes=[tmk])
            S.call("dve", "scalar_tensor_tensor", out=tm[:], in0=gb[:, o - 1:o - 1 + TT], scalar=cw[:, i, 1:2], in1=tm[:],
                   op0=ALU.mult, op1=ALU.add, reads=[(gbk, t), (gbk, t - 1), tmk, "f_cw"], writes=[tmk])
            S.call("dve", "scalar_tensor_tensor", out=tm[:], in0=gb[:, o - 2:o - 2 + TT], scalar=cw[:, i, 0:1], in1=tm[:],
                   op0=ALU.mult, op1=ALU.add, reads=[(gbk, t), (gbk, t - 1), tmk, "f_cw"], writes=[tmk])
            ge, gek = ge_r.next()
            S.act(ge[:], tm[:], AF.Gelu_apprx_tanh, reads=[tmk], writes=[gek])
            S.call("dve", "tensor_tensor", out=ast[:, t * TT:(t + 1) * TT], in0=ge[:], in1=pu[:], op=ALU.mult,
                   reads=[gek, puk], writes=[ask])
        S.dma("sp", aT_d[i * 128:(i + 1) * 128, :], ast[:], reads=[ask])


def conv_w_pc(cw):
    K_, Cn = cw.shape
    return np.ascontiguousarray(cw.reshape(K_, Cn // 128, 128).transpose(2, 1, 0).astype(np.float32))


def build_L3(with_up=True, with_down=True):
    nc = bass.Bass("TRN2", target_bir_lowering=False)
    S = Sched(nc)
    xT_d = nc.dram_tensor("xT", [D, NTOK], F32, kind="ExternalInput").ap()
    xh_d = nc.dram_tensor("xh", [D, 2], F32, kind="ExternalInput").ap()
    flag_d = nc.dram_tensor("flag", [128, 1], F32, kind="ExternalInput").ap()
    modp_d, gds = decl_vec_inputs(nc)
    wup_d = nc.dram_tensor("w_up", [D, 2 * DFF], F32, kind="ExternalInput").ap()
    wdn_d = nc.dram_tensor("w_down", [DFF, D], F32, kind="ExternalInput").ap()
    cw_d = nc.dram_tensor("conv_w", [128, 44, 3], F32, kind="ExternalInput").ap()
    cb_d = nc.dram_tensor("conv_b", [128, 44], F32, kind="ExternalInput").ap()
    aT_d = nc.dram_tensor("aT", [DFF, NTOK], BF16, kind="Internal").ap()
    xo_d = nc.dram_tensor("xoT", [D, NTOK], F32, kind="ExternalOutput").ap()
    C = Common(S, nxt=1)
    V = emit_modvecs(S, modp_d, *gds)
    S.phase_begin()
    emit_ffn_up(S, C, xT_d, xh_d, flag_d, V["gm_f"], V["sh_f"], [V["k_f"], "modp"], wup_d, cw_d, cb_d, aT_d)
    S.phase_end()
    S.phase_begin()
    emit_G2(S, C, aT_d, 44, wdn_d, xT_d, xo_d, V["gg_f"], V["k_gf"], "fd", nabuf=1)
    S.phase_end()
    S.emit()
    S.close()
    return nc


def emit_proj_tm(S, hT, wt, wk, col0, N, psr, stage, skey, hkey):
    for tt in range(16):
        ps, pk = psr.next()
        for c in range(16):
            S.mm(ps[:, 0:N], hT[:, c, tt * 128:(tt + 1) * 128], wt[:, c, col0:col0 + N], c == 0, c == 15,
                 reads=[wk, (hkey, tt // 4)], writes=[pk])
        S.call("dve", "tensor_copy", out=stage[:, tt, 0:N], in_=ps[:, 0:N], reads=[pk], writes=[skey])


def emit_L4(S, C, xT_d, gm, sh, vkeys, w_d, qkT_d, vc_d, oc_d, g_d, qdT_d, kdT_d, vd_d):
    hT = S.sbuf("p1_hT", [128, 16, NTOK], BF16)
    emit_norm_hT(S, C, xT_d, gm, sh, vkeys, hT, hkey="p1_hT")
    wv = w_d.rearrange("(c p) n -> p c n", p=128)
    wr = Rot(S, "p1_w", 2, [128, 16, 512], BF16)
    psr = C.banks.rot([0, 1, 2, 3])
    fst = Rot(S, "p1_fst", 1, [128, NTOK], F32)
    qst = Rot(S, "p1_qst", 2, [64, NTOK], BF16)
    tst = Rot(S, "p1_tst", 1, [128, 16, 512], BF16)
    gst = S.sbuf("p1_gst", [128, 16, 8], F32)
    for g in range(2):
        wt, wk = wr.next()
        S.dma("pool", wt[:], wv[:, :, g * 512:(g + 1) * 512], writes=[wk])
        for j in range(4):
            st, sk = fst.next()
            emit_proj_fm(S, hT, wt, wk, j * 128, 128, psr, st, sk, "dve", hkey="p1_hT")
            S.dma("sp", qkT_d[(g * 4 + j) * 128:(g * 4 + j + 1) * 128, :], st[:], reads=[sk])
    for dst, c0 in ((vc_d, 1024), (oc_d, 2048)):
        for g in range(2):
            wt, wk = wr.next()
            S.dma("pool", wt[:], wv[:, :, c0 + g * 512:c0 + (g + 1) * 512], writes=[wk])
            st, sk = tst.next()
            emit_proj_tm(S, hT, wt, wk, 0, 512, psr, st, sk, "p1_hT")
            S.dma("sp", dst.rearrange("(t p) n -> p t n", p=128)[:, :, g * 512:(g + 1) * 512], st[:], reads=[sk])
    wt, wk = wr.next()
    S.dma("pool", wt[:, :, 0:8], wv[:, :, 3072:3080], writes=[wk])
    for tt in range(16):
        ps, pk = psr.next()
        for c in range(16):
            S.mm(ps[:, 0:8], hT[:, c, tt * 128:(tt + 1) * 128], wt[:, c, 0:8], c == 0, c == 15,
                 reads=[wk, ("p1_hT", tt // 4)], writes=[pk])
        S.call("dve", "tensor_copy", out=gst[:, tt, :], in_=ps[:, 0:8], reads=[pk], writes=["p1_gst"])
    S.dma("sp", g_d.rearrange("(t p) n -> p t n", p=128), gst[:], reads=["p1_gst"])
    for g in range(2):
        wt, wk = wr.next()
        S.dma("pool", wt[:], wv[:, :, 3080 + g * 512:3080 + (g + 1) * 512], writes=[wk])
        for hh in range(8):
            st, sk = qst.next()
            emit_proj_fm(S, hT, wt, wk, hh * 64, 64, psr, st, sk, "act", scale=0.125, hkey="p1_hT")
            S.dma("sp", qdT_d[g * 8 + hh], st[:], reads=[sk])
    wt, wk = wr.next()
    S.dma("pool", wt[:], wv[:, :, 4104:4616], writes=[wk])
    for j in range(4):
        st, sk = qst.next()
        emit_proj_fm(S, hT, wt, wk, j * 64, 64, psr, st, sk, "act", hkey="p1_hT")
        S.dma("sp", kdT_d[j], st[:], reads=[sk])
    st, sk = tst.next()
    emit_proj_tm(S, hT, wt, wk, 256, 256, psr, st, sk, "p1_hT")
    S.dma("sp", vd_d.rearrange("(t p) n -> p t n", p=128), st[:, :, 0:256], reads=[sk])


def build_L4():
    nc = bass.Bass("TRN2", target_bir_lowering=False)
    S = Sched(nc)
    xT_d = nc.dram_tensor("xT", [D, NTOK], F32, kind="ExternalInput").ap()
    modp_d, gds = decl_vec_inputs(nc)
    w_d = nc.dram_tensor("w_in", [D, 4616], F32, kind="ExternalInput").ap()
    qkT_d = nc.dram_tensor("qkT", [1024, NTOK], F32, kind="ExternalOutput").ap()
    vc_d = nc.dram_tensor("vc", [NTOK, 1024], BF16, kind="ExternalOutput").ap()
    oc_d = nc.dram_tensor("oc", [NTOK, 1024], BF16, kind="ExternalOutput").ap()
    g_d = nc.dram_tensor("gc", [NTOK, 8], F32, kind="ExternalOutput").ap()
    qdT_d = nc.dram_tensor("qdT", [16, 64, NTOK], BF16, kind="ExternalOutput").ap()
    kdT_d = nc.dram_tensor("kdT", [4, 64, NTOK], BF16, kind="ExternalOutput").ap()
    vd_d = nc.dram_tensor("vd", [NTOK, 256], BF16, kind="ExternalOutput").ap()
    C = Common(S, nxt=1)
    V = emit_modvecs(S, modp_d, *gds)
    emit_L4(S, C, xT_d, V["gm_m"], V["sh_m"], [V["k_m"], "modp"], w_d, qkT_d, vc_d, oc_d, g_d, qdT_d, kdT_d, vd_d)
    S.emit()
    S.close()
    return nc


TSEQ = 4096
NCH = TSEQ // 64


def emit_mlstm(S, B, qk_d, cw_d, cb_d, vc_d, oc_d, gi_d, gf_d, gb_d, mhn_d, cst_d, ident_d, hcT_d, nch=NCH, dbg=9):
    qT = S.sbuf("m_qT", [128, 2, TSEQ], BF16)
    kT = S.sbuf("m_kT", [128, 2, TSEQ], BF16)
    cw = S.sbuf("m_cw", [128, 4, 4], F32)
    cb = S.sbuf("m_cb", [128, 4], F32)
    gi = S.sbuf("m_gi", [64, NCH, 2], F32)
    gf = S.sbuf("m_gf", [64, NCH, 2], F32)
    gb = S.sbuf("m_gb", [64, 4], F32)
    mhn = S.sbuf("m_mhn", [64, 2, 256], F32)
    cst = S.sbuf("m_cst", [64, 3, 128], F32)
    ident = S.sbuf("m_ident", [128, 128], BF16)
    ea = S.sbuf("m_ea", [64, NCH, 2], F32)
    ew = S.sbuf("m_ew", [64, NCH, 2], F32)
    eb = S.sbuf("m_eb", [64, NCH, 2], F32)
    ebt = S.sbuf("m_ebt", [128, NCH, 2], F32)
    bb = S.sbuf("m_b", [64, NCH, 2], F32)
    for t_, d_, k_ in ((cw, cw_d, "m_cw"), (cb, cb_d, "m_cb"), (gi, gi_d, "m_gi"), (gf, gf_d, "m_gf"), (gb, gb_d, "m_gb"),
                       (mhn, mhn_d, "m_mhn"), (cst, cst_d, "m_cst"), (ident, ident_d, "m_ident")):
        S.dma("sp", t_[:], d_, writes=[k_])
    if dbg < 1:
        return
    for h in range(2):
        S.call("dve", "tensor_scalar", out=gf[:, :, h], in0=gf[:, :, h], scalar1=gb[:, 2 + h:3 + h], scalar2=None, op0=ALU.add,
               reads=["m_gf", "m_gb"], writes=["m_gf"])
        S.call("dve", "tensor_scalar", out=gi[:, :, h], in0=gi[:, :, h], scalar1=gb[:, h:h + 1], scalar2=None, op0=ALU.add,
               reads=["m_gi", "m_gb"], writes=["m_gi"])
    S.act(gf[:], gf[:], AF.Exp, scale=-1.0, reads=["m_gf"], writes=["m_gf"])
    S.act(gf[:], gf[:], AF.Ln, bias=1.0, reads=["m_gf"], writes=["m_gf"])
    gfv = gf[:].rearrange("p c h -> p (c h)")
    if dbg < 2:
        return
    pb, pbk = B.rot([0]).next()
    pt, ptk = B.rot([1]).next()
    S.mm(pb[0:64, 0:128], cst[:, 0, 0:64], gfv, True, True, reads=["m_cst", "m_gf"], writes=[pbk])
    if dbg < 2.2:
        return
    S.mm(pt[:, 0:128], cst[:, 1, :], gfv, True, True, reads=["m_cst", "m_gf"], writes=[ptk])
    if dbg < 2.4:
        return
    flat = lambda t: t[:].rearrange("p c h -> p (c h)")
    S.call("dve", "tensor_copy", out=flat(bb), in_=pb[0:64, 0:128], reads=[pbk], writes=["m_b"])
    S.act(flat(eb), pb[0:64, 0:128], AF.Exp, reads=[pbk], writes=["m_eb"])
    S.act(flat(ebt), pt[:, 0:128], AF.Exp, reads=[ptk], writes=["m_ebt"])
    if dbg < 2.6:
        return
    S.call("dve", "tensor_tensor", out=flat(gi), in0=flat(gi), in1=flat(bb), op=ALU.subtract, reads=["m_gi", "m_b"], writes=["m_gi"])
    S.act(flat(ea), flat(gi), AF.Exp, reads=["m_gi"], writes=["m_ea"])
    S.call("dve", "tensor_tensor", out=flat(gi), in0=flat(gi), in1=pt[0:64, 0:128], op=ALU.add, reads=["m_gi", ptk], writes=["m_gi"])
    S.act(flat(ew), flat(gi), AF.Exp, reads=["m_gi"], writes=["m_ew"])
    if dbg < 3:
        return
    xr = Rot(S, "m_x", 2, [128, TT + 3], F32)
    tm = Rot(S, "m_tm", 2, [128, TT], F32)
    for r in range(4):
        dst = (qT if r < 2 else kT)
        h = r % 2
        for t in range(TSEQ // TT):
            x, xk = xr.next()
            S.dma("sp", x[:], qk_d[r, :, t * TT:t * TT + TT + 3], writes=[xk])
            y, yk = tm.next()
            S.call("dve", "tensor_scalar", out=y[:], in0=x[:, 3:TT + 3], scalar1=cw[:, r, 3:4], scalar2=cb[:, r:r + 1],
                   op0=ALU.mult, op1=ALU.add, reads=[xk, "m_cw", "m_cb"], writes=[yk])
            for j in range(3):
                S.call("dve", "scalar_tensor_tensor", out=y[:], in0=x[:, j:j + TT], scalar=cw[:, r, j:j + 1], in1=y[:],
                       op0=ALU.mult, op1=ALU.add, reads=[xk, yk, "m_cw"], writes=[yk])
            if r < 2:
                S.act(dst[:, h, t * TT:(t + 1) * TT], y[:], AF.Silu, reads=[yk], writes=[("m_qk", r, t)])
            else:
                S.act(y[:], y[:], AF.Silu, reads=[yk], writes=[yk])
                S.call("dve", "tensor_scalar", out=dst[:, h, t * TT:(t + 1) * TT], in0=y[:], scalar1=128.0 ** -0.5, scalar2=None,
                       op0=ALU.mult, reads=[yk], writes=[("m_qk", r, t)])
    Cst = [S.sbuf(f"m_C{h}", [128, 257], F32) for h in range(2)]
    Cbf = [S.sbuf(f"m_Cb{h}", [128, 257], BF16) for h in range(2)]
    for h in range(2):
        S.call("dve", "memset", Cst[h][:], 0.0, writes=[("m_C", h)])
        S.call("dve", "memset", Cbf[h][:], 0.0, writes=[("m_Cb", h)])
    v_r = Rot(S, "m_v", 2, [64, 8, 2, 257], BF16)
    o_r = Rot(S, "m_o", 2, [64, 8, 512], BF16)
    kw_r = Rot(S, "m_kw", 2, [64, 128], BF16)
    pp_r = Rot(S, "m_pp", 2, [64, 64], BF16)
    ns_r = Rot(S, "m_ns", 2, [64, 2, 257], F32)
    sm_r = Rot(S, "m_sm", 2, [64, 8], F32)
    hn_r = Rot(S, "m_hn", 2, [64, 2, 256], F32)
    sg_r = Rot(S, "m_sg", 2, [64, 512], F32)
    hg_r = Rot(S, "m_hg", 2, [64, 512], BF16)
    hs_r = Rot(S, "m_hs", 2, [128, 4, TT], BF16)
    junk = S.sbuf("m_junk", [64, 256], F32)
    ps_kt = B.rot([0, 1])
    ps_s = B.rot([2, 3])
    ps_n = B.rot([4, 5])
    ps_c = B.rot([6])
    ps_t = B.rot([7])
    vv = vc_d.rearrange("(c p) h d -> p c h d", p=64)
    ov = oc_d.rearrange("(c p) n -> p c n", p=64)
    for c in range(nch):
        cc = c % 8
        if cc == 0:
            v, vk = v_r.next()
            S.call("dve", "memset", v[:, :, :, 256:257], 1.0, writes=[vk])
            for h in range(2):
                S.dma("sp", v[:, :, h, 0:256], vv[:, c:c + 8, h, :], reads=[vk], writes=[vk])
            o, ok = o_r.next()
            S.dma("sp", o[:], ov[:, c:c + 8, :], writes=[ok])
            hs, hsk = hs_r.next()
        tok = slice(c * 64, (c + 1) * 64)
        tq = (c * 64) // TT
        ns, nsk = ns_r.next()
        for h in range(2):
            qk_keys = [("m_qk", h, tq), ("m_qk", 2 + h, tq)]
            pk_, pkk = ps_kt.next()
            pkv = pk_[0:64, 0:64].bitcast(BF16)
            S.op("pe", (lambda e, o_=pkv, i_=kT[:, h, tok]: e.transpose(o_, i_, ident[:])),
                 reads=[("m_qk", 2 + h, tq), "m_ident"], writes=[pkk])
            kw, kwk = kw_r.next()
            S.call("dve", "tensor_scalar", out=kw[:], in0=pkv, scalar1=ew[:, c, h:h + 1], scalar2=None, op0=ALU.mult,
                   reads=[pkk, "m_ew"], writes=[kwk])
            pss, psk = ps_s.next()
            S.mm(pss[0:64, 0:64], kT[:, h, tok], qT[:, h, tok], True, True, reads=qk_keys, writes=[psk])
            pp, ppk = pp_r.next()
            S.call("dve", "scalar_tensor_tensor", out=pp[:], in0=pss[0:64, 0:64], scalar=ea[:, c, h:h + 1], in1=cst[:, 2, 0:64],
                   op0=ALU.mult, op1=ALU.mult, reads=[psk, "m_ea", "m_cst"], writes=[ppk])
            pn, pnk = ps_n.next()
            S.mm(pn[0:64, 0:257], pp[:], v[:, cc, h, :], True, False, reads=[ppk, vk], writes=[pnk])
            S.mm(pn[0:64, 0:257], qT[:, h, tok], Cbf[h][:], False, True, reads=qk_keys + [("m_Cb", h)], writes=[pnk])
            pc, pck = ps_c.next()
            S.mm(pc[:, 0:257], kw[:], v[:, cc, h, :], True, True, reads=[kwk, vk], writes=[pck])
            S.call("dve", "scalar_tensor_tensor", out=Cst[h][:], in0=Cst[h][:], scalar=ebt[:, c, h:h + 1], in1=pc[:, 0:257],
                   op0=ALU.mult, op1=ALU.add, reads=[("m_C", h), "m_ebt", pck], writes=[("m_C", h)])
            S.call("act", "copy", out=Cbf[h][:], in_=Cst[h][:], reads=[("m_C", h)], writes=[("m_Cb", h)])
            S.call("act", "copy", out=ns[:, h, :], in_=pn[0:64, 0:257], reads=[pnk], writes=[nsk])
        sm, smk = sm_r.next()
        S.call("dve", "tensor_tensor", out=sm[:, 0:2], in0=ns[:, :, 256], in1=eb[:, c, :], op=ALU.mult, reads=[nsk, "m_eb"], writes=[smk])
        S.act(sm[:, 0:2], sm[:, 0:2], AF.Abs, reads=[smk], writes=[smk])
        S.call("dve", "tensor_scalar_max", out=sm[:, 0:2], in0=sm[:, 0:2], scalar1=1.0, reads=[smk], writes=[smk])
        S.call("dve", "reciprocal", out=sm[:, 0:2], in_=sm[:, 0:2], reads=[smk], writes=[smk])
        S.call("dve", "tensor_tensor", out=sm[:, 0:2], in0=sm[:, 0:2], in1=eb[:, c, :], op=ALU.mult, reads=[smk, "m_eb"], writes=[smk])
        for h in range(2):
            S.act(junk[:], ns[:, h, 0:256], AF.Square, scale=sm[:, h:h + 1], accum_out=sm[:, 2 + h:3 + h],
                  reads=[nsk, smk], writes=[smk, "m_junk"])
        S.act(sm[:, 4:6], sm[:, 2:4], AF.Sqrt, scale=1.0 / 256, bias=EPS_AP[0][0:64, :], reads=[smk, "epsc"], writes=[smk])
        S.call("dve", "reciprocal", out=sm[:, 4:6], in_=sm[:, 4:6], reads=[smk], writes=[smk])
        S.call("dve", "tensor_tensor", out=sm[:, 6:8], in0=sm[:, 4:6], in1=sm[:, 0:2], op=ALU.mult, reads=[smk], writes=[smk])
        hn, hnk = hn_r.next()
        S.call("dve", "tensor_tensor", out=hn[:], in0=ns[:, :, 0:256], in1=sm[:, 6:8].unsqueeze(2).to_broadcast([64, 2, 256]),
               op=ALU.mult, reads=[nsk, smk], writes=[hnk])
        S.call("dve", "tensor_tensor", out=hn[:], in0=hn[:], in1=mhn[:], op=ALU.mult, reads=[hnk, "m_mhn"], writes=[hnk])
        sg, sgk = sg_r.next()
        S.act(sg[:], o[:, cc, :], AF.Sigmoid, reads=[ok], writes=[sgk])
        hg, hgk = hg_r.next()
        S.call("dve", "tensor_tensor", out=hg[:], in0=hn[:].rearrange("p h d -> p (h d)"), in1=sg[:], op=ALU.mult,
               reads=[hnk, sgk], writes=[hgk])
        ptt, pttk = ps_t.next()
        ptv = ptt[:, 0:128].bitcast(BF16).rearrange("p (f t) -> p f t", f=4)
        for f in range(4):
            S.op("pe", (lambda e, o_=ptv[:, f, :], i_=hg[:, f * 128:(f + 1) * 128]: e.transpose(o_, i_, ident[0:64, 0:64])),
                 reads=[hgk, "m_ident"], writes=[pttk])
        S.call("act", "copy", out=hs[:, :, cc * 64:(cc + 1) * 64], in_=ptv, reads=[pttk], writes=[hsk])
        if cc == 7:
            t0 = (c - 7) * 64
            S.dma("sp", hcT_d[:, t0:t0 + TT].rearrange("(f p) t -> p f t", p=128), hs[:], reads=[hsk])


EPS_AP = [None]


def mlstm_consts():
    j = np.arange(64)[:, None]
    l = np.arange(64)[None, :]
    c = np.zeros((64, 3, 128), np.float32)
    c[:, 0, :64] = -1.0 * (j <= l)
    c[:, 1, :] = -1.0
    c[:, 2, :64] = (j <= l)
    return c


def build_L5a(nch=NCH, dbg=9):
    nc = bass.Bass("TRN2", target_bir_lowering=False)
    S = Sched(nc)
    qk_d = nc.dram_tensor("qk", [4, 128, TSEQ + 3], F32, kind="ExternalInput").ap()
    cw_d = nc.dram_tensor("cw", [128, 4, 4], F32, kind="ExternalInput").ap()
    cb_d = nc.dram_tensor("cb", [128, 4], F32, kind="ExternalInput").ap()
    vc_d = nc.dram_tensor("vc", [TSEQ, 2, 256], BF16, kind="ExternalInput").ap()
    oc_d = nc.dram_tensor("oc", [TSEQ, 512], BF16, kind="ExternalInput").ap()
    gi_d = nc.dram_tensor("gi", [64, NCH, 2], F32, kind="ExternalInput").ap()
    gf_d = nc.dram_tensor("gf", [64, NCH, 2], F32, kind="ExternalInput").ap()
    gb_d = nc.dram_tensor("gb", [64, 4], F32, kind="ExternalInput").ap()
    mhn_d = nc.dram_tensor("mhn", [64, 2, 256], F32, kind="ExternalInput").ap()
    cst_d = nc.dram_tensor("mcst", [64, 3, 128], F32, kind="ExternalInput").ap()
    ident_d = nc.dram_tensor("ident", [128, 128], BF16, kind="ExternalInput").ap()
    hcT_d = nc.dram_tensor("hcT", [512, TSEQ], BF16, kind="ExternalOutput").ap()
    B = Banks(S)
    eps = S.sbuf("eps_c", [128, 1], F32)
    S.call("dve", "memset", eps[:], EPS, writes=["epsc"])
    EPS_AP[0] = eps
    emit_mlstm(S, B, qk_d, cw_d, cb_d, vc_d, oc_d, gi_d, gf_d, gb_d, mhn_d, cst_d, ident_d, hcT_d, nch=nch, dbg=dbg)
    S.emit()
    S.close()
    return nc


def mlstm_inputs(inp, qkT_full, vc_full, oc_full, gc_full, hh):
    heads = (2 * hh, 2 * hh + 1)
    rows = [h * 128 for h in heads] + [512 + h * 128 for h in heads]
    qk = np.zeros((4, 128, TSEQ + 3), np.float32)
    for r, r0 in enumerate(rows):
        qk[r, :, 3:] = qkT_full[r0:r0 + 128]
    cwf = inp["od_conv_w"][0]
    cw = np.stack([cwf[:, r0:r0 + 128].T for r0 in rows], axis=1)
    cb = np.stack([inp["od_conv_b"][0][r0:r0 + 128] for r0 in rows], axis=1)
    vc = np.ascontiguousarray(vc_full.reshape(TSEQ, 4, 256)[:, heads[0]:heads[0] + 2, :])
    oc = np.ascontiguousarray(oc_full[:, heads[0] * 256:heads[0] * 256 + 512])
    g = gc_full.reshape(NCH, 64, 8).transpose(1, 0, 2)
    gi = np.ascontiguousarray(g[:, :, heads[0]:heads[0] + 2])
    gf = np.ascontiguousarray(g[:, :, 4 + heads[0]:4 + heads[0] + 2])
    gbv = inp["od_gate_b"][0]
    gb = np.broadcast_to(np.array([gbv[heads[0]], gbv[heads[1]], gbv[4 + heads[0]], gbv[4 + heads[1]]], np.float32)[None], (64, 4))
    mhn = np.broadcast_to(inp["od_mh_norm"][0].reshape(4, 256)[heads[0]:heads[0] + 2][None], (64, 2, 256))
    return dict(qk=qk, cw=np.ascontiguousarray(cw.astype(np.float32)), cb=np.ascontiguousarray(cb.astype(np.float32)), vc=vc, oc=oc,
                gi=gi.astype(np.float32), gf=gf.astype(np.float32), gb=np.ascontiguousarray(gb), mhn=np.ascontiguousarray(mhn.astype(np.float32)),
                mcst=mlstm_consts(), ident=np.eye(128, dtype=np.float32).astype(NPBF))


def _split3(x):
    x = np.asarray(x, np.float64)
    hi = x.astype(NPBF)
    r1 = x - hi.astype(np.float64)
    mid = r1.astype(NPBF)
    r2 = r1 - mid.astype(np.float64)
    lo = r2.astype(NPBF)
    return hi, mid, lo


def moba_consts(hh):
    s = np.arange(TSEQ)
    ka = np.zeros((24, TSEQ), np.float32)
    ka[0:16] = (s[None, :] // 256 == np.arange(16)[:, None])
    ka[16:19] = (128 * (s // 128))[None]
    ka[19:21] = (s % 128)[None]
    ka[21:24] = 1.0
    qa = np.zeros((8, 8, TSEQ), NPBF)
    for h in range(8):
        slope = 2.0 ** (-8.0 * (8 * hh + h + 1) / 16)
        a, b, c = _split3(slope)
        qa[h, 0], qa[h, 1], qa[h, 2] = a, b, c
        qa[h, 3], qa[h, 4] = a, b
        a, b, c = _split3(-slope * s.astype(np.float64))
        qa[h, 5], qa[h, 6], qa[h, 7] = a, b, c
    tri = np.where(np.arange(128)[:, None] <= np.arange(128)[None, :], 0.0, -30000.0).astype(np.float32)
    return ka.astype(NPBF), qa, np.ascontiguousarray(np.broadcast_to(tri[:, None, :], (128, 4, 128)))


def emit_moba(S, B, qdT_d, kdT_d, vd_d, kac_d, qac_d, tri_d, ident_d, hdT_d, ntile=32):
    Ka = S.sbuf("b_Ka", [88, 2, TSEQ], BF16)
    Qa = S.sbuf("b_Qa", [88, 8, TSEQ], BF16)
    v_sb = S.sbuf("b_v", [128, 32, 2, 65], BF16)
    tri = S.sbuf("b_tri", [128, 4, 128], F32)
    ident = S.sbuf("b_ident", [128, 128], BF16)
    ksum = S.sbuf("b_ksum", [64, 2, 16], F32)
    kmh = S.sbuf("b_kmh", [64, 2, 16], BF16)
    kml = S.sbuf("b_kml", [64, 2, 16], BF16)
    kmf = S.sbuf("b_kmf", [64, 2, 16], F32)
    S.dma("sp", Ka[0:64, :, :], kdT_d.rearrange("j d t -> d j t"), writes=["b_Ka"])
    for j in range(2):
        S.dma("sp", Ka[64:88, j, :], kac_d, writes=[("b_Kc", j)])
    S.dma("sp", Qa[0:64, :, :], qdT_d.rearrange("h d t -> d h t"), writes=["b_Qq"])
    S.dma("sp", Qa[80:88, :, :], qac_d.rearrange("h r t -> r h t"), writes=["b_Qc"])
    S.call("dve", "memset", Qa[64:80, :, :], 0.0, writes=[("b_Qm", i) for i in range(32)])
    S.call("dve", "memset", v_sb[:], 1.0, writes=["b_v"])
    for j in range(2):
        S.dma("sp", v_sb[:, :, j, 0:64], vd_d.rearrange("(c p) j d -> p c j d", p=128)[:, :, j, :], reads=["b_v"], writes=["b_v"])
    S.dma("sp", tri[:], tri_d, writes=["b_tri"])
    S.dma("sp", ident[:], ident_d, writes=["b_ident"])
    for j in range(2):
        S.call("dve", "tensor_reduce", out=ksum[:, j, :], in_=Ka[0:64, j, :].rearrange("p (n s) -> p n s", s=256), axis=AX.X,
               op=ALU.add, reads=["b_Ka"], writes=["b_ksum"])
    S.call("dve", "tensor_scalar", out=ksum[:], in0=ksum[:], scalar1=1.0 / 256, scalar2=None, op0=ALU.mult, reads=["b_ksum"], writes=["b_ksum"])
    S.call("dve", "tensor_copy", out=kmh[:], in_=ksum[:], reads=["b_ksum"], writes=["b_kmh"])
    S.call("dve", "tensor_copy", out=kmf[:], in_=kmh[:], reads=["b_kmh"], writes=["b_kmf"])
    S.call("dve", "tensor_tensor", out=kmf[:], in0=ksum[:], in1=kmf[:], op=ALU.subtract, reads=["b_ksum", "b_kmf"], writes=["b_kmf"])
    S.call("dve", "tensor_copy", out=kml[:], in_=kmf[:], reads=["b_kmf"], writes=["b_kml"])
    gs_r = Rot(S, "b_gs", 2, [128, 4, 16], F32)
    for g_ in gs_r.tiles:
        pass
    for t_, k_ in zip(gs_r.tiles, gs_r.keys):
        S.call("dve", "memset", t_[:], -1e30, writes=[k_])
    t8_r = Rot(S, "b_t8", 2, [128, 4, 8], F32)
    mt_r = Rot(S, "b_mt", 2, [128, 4, 16], F32)
    mb_r = Rot(S, "b_mb", 2, [128, 4, 80], BF16)
    for t_, k_ in zip(mb_r.tiles, mb_r.keys):
        S.call("dve", "memset", t_[:], 0.0, writes=[k_])
    ef_r = Rot(S, "b_ef", 2, [128, 512], F32)
    e_r = Rot(S, "b_e", 3, [128, 512], BF16)
    dn_r = Rot(S, "b_dn", 2, [128, 4], F32)
    at_r = Rot(S, "b_at", 2, [128, 4, 64], BF16)
    hs_r = Rot(S, "b_hs", 4, [128, 2, TT], BF16)
    ps_g = B.rot([0])
    ps_m = B.rot([1])
    ps_s = B.rot([2, 3])
    ps_o = B.rot([4, 5])
    ps_t = B.rot([6])
    hs_cur = {}
    for i in range(ntile):
        tq = slice(i * 128, (i + 1) * 128)
        nb = i // 2
        for j in range(2):
            qkeys = ["b_Qq", "b_Qc", ("b_Qm", i)]
            if nb > 3:
                pg, pgk = ps_g.next()
                pgv = pg[:, 0:64].rearrange("p (h n) -> p h n", h=4)
                for h4 in range(4):
                    S.mm(pgv[:, h4, :], Qa[0:64, 4 * j + h4, tq], kmh[:, j, :], True, False, reads=["b_Qq", "b_kmh"], writes=[pgk])
                    S.mm(pgv[:, h4, :], Qa[0:64, 4 * j + h4, tq], kml[:, j, :], False, True, reads=["b_Qq", "b_kml"], writes=[pgk])
                gs, gsk = gs_r.next()
                S.call("act", "copy", out=gs[:, :, 0:nb], in_=pgv[:, :, 0:nb], reads=[pgk], writes=[gsk])
                t8, t8k = t8_r.next()
                mt, mtk = mt_r.next()
                mb, mbk = mb_r.next()
                for h4 in range(4):
                    S.call("dve", "max", out=t8[:, h4, :], in_=gs[:, h4, :], reads=[gsk], writes=[t8k])
                    S.call("dve", "tensor_scalar", out=mt[:, h4, :], in0=gs[:, h4, :], scalar1=t8[:, h4, 2:3], scalar2=1.0,
                           op0=ALU.is_ge, op1=ALU.subtract, reads=[gsk, t8k], writes=[mtk])
                S.call("dve", "tensor_scalar", out=mb[:, :, 64:80], in0=mt[:], scalar1=30000.0, scalar2=None, op0=ALU.mult,
                       reads=[mtk], writes=[mbk])
                S.call("dve", "memset", mb[:, :, 64 + nb:80], 0.0, reads=[mbk], writes=[mbk])
                pm, pmk = ps_m.next()
                pmv = pm[0:80, :].bitcast(BF16)[:, 0:512].rearrange("p (h t) -> p h t", h=4)
                for h4 in range(4):
                    S.op("pe", (lambda e, o_=pmv[:, h4, :], i_=mb[:, h4, :]: e.transpose(o_, i_, ident[:])),
                         reads=[mbk, "b_ident"], writes=[pmk])
                S.call("act", "copy", out=Qa[64:80, 4 * j:4 * j + 4, tq], in_=pmv[64:80, :, :], reads=[pmk], writes=[("b_Qm", i)])
            po, pok = ps_o.next()
            pov = po[:, 0:260].rearrange("p (h d) -> p h d", d=65)
            for c in range(i + 1):
                ps, psk = ps_s.next()
                S.mm(ps[:].rearrange("p (h q) -> p h q", h=4), Ka[:, j, c * 128:(c + 1) * 128], Qa[:, 4 * j:4 * j + 4, tq], True, True,
                     reads=["b_Ka", ("b_Kc", j)] + qkeys, writes=[psk])
                e, ek = e_r.next()
                if c == i:
                    ef, efk = ef_r.next()
                    S.call("dve", "tensor_tensor", out=ef[:], in0=ps[:], in1=tri[:].rearrange("p h q -> p (h q)"), op=ALU.add,
                           reads=[psk, "b_tri"], writes=[efk])
                    S.act(e[:], ef[:], AF.Exp, reads=[efk], writes=[ek])
                else:
                    S.act(e[:], ps[:], AF.Exp, reads=[psk], writes=[ek])
                for h4 in range(4):
                    S.mm(pov[:, h4, :], e[:, h4 * 128:(h4 + 1) * 128], v_sb[:, c, j, :], (c == 0 and h4 == 0), (c == i and h4 == 3),
                         reads=[ek, "b_v"], writes=[pok])
            dn, dk = dn_r.next()
            S.call("dve", "reciprocal", out=dn[:], in_=pov[:, :, 64], reads=[pok], writes=[dk])
            at, ak = at_r.next()
            S.call("dve", "tensor_tensor", out=at[:], in0=pov[:, :, 0:64], in1=dn[:].unsqueeze(2).to_broadcast([128, 4, 64]),
                   op=ALU.mult, reads=[pok, dk], writes=[ak])
            pt, ptk = ps_t.next()
            ptv = pt[:, 0:128].bitcast(BF16).rearrange("p (f t) -> p f t", f=2)
            atv = at[:].rearrange("p h d -> p (h d)")
            for f in range(2):
                S.op("pe", (lambda e_, o_=ptv[:, f, :], i_=atv[:, f * 128:(f + 1) * 128]: e_.transpose(o_, i_, ident[:])),
                     reads=[ak, "b_ident"], writes=[ptk])
            if i % 4 == 0:
                hs_cur[j] = hs_r.next()
            hs, hsk = hs_cur[j]
            S.call("act", "copy", out=hs[:, :, (i % 4) * 128:(i % 4 + 1) * 128], in_=ptv, reads=[ptk], writes=[hsk])
            if i % 4 == 3:
                t0 = (i - 3) * 128
                S.dma("sp", hdT_d[j * 256:(j + 1) * 256, t0:t0 + TT].rearrange("(f p) t -> p f t", p=128), hs[:], reads=[hsk])


def build_L5b(ntile=32):
    nc = bass.Bass("TRN2", target_bir_lowering=False)
    S = Sched(nc)
    qdT_d = nc.dram_tensor("qdT", [8, 64, TSEQ], BF16, kind="ExternalInput").ap()
    kdT_d = nc.dram_tensor("kdT", [2, 64, TSEQ], BF16, kind="ExternalInput").ap()
    vd_d = nc.dram_tensor("vd", [TSEQ, 2, 64], BF16, kind="ExternalInput").ap()
    kac_d = nc.dram_tensor("kac", [24, TSEQ], BF16, kind="ExternalInput").ap()
    qac_d = nc.dram_tensor("qac", [8, 8, TSEQ], BF16, kind="ExternalInput").ap()
    tri_d = nc.dram_tensor("tri", [128, 4, 128], F32, kind="ExternalInput").ap()
    ident_d = nc.dram_tensor("ident", [128, 128], BF16, kind="ExternalInput").ap()
    hdT_d = nc.dram_tensor("hdT", [512, TSEQ], BF16, kind="ExternalOutput").ap()
    B = Banks(S)
    emit_moba(S, B, qdT_d, kdT_d, vd_d, kac_d, qac_d, tri_d, ident_d, hdT_d, ntile=ntile)
    S.emit()
    S.close()
    return nc


def moba_inputs(qdT_full, kdT_full, vd_full, hh):
    ka, qa, tri = moba_consts(hh)
    return dict(qdT=np.ascontiguousarray(qdT_full[8 * hh:8 * hh + 8]), kdT=np.ascontiguousarray(kdT_full[2 * hh:2 * hh + 2]),
                vd=np.ascontiguousarray(vd_full.reshape(TSEQ, 4, 64)[:, 2 * hh:2 * hh + 2, :]), kac=ka, qac=qa, tri=tri,
                ident=np.eye(128, dtype=np.float32).astype(NPBF))


def _run(nc, maps):
    return run_bass_kernel_spmd(nc, maps, core_ids=list(range(8))).results


def _ident():
    return np.eye(128, dtype=np.float32).astype(NPBF)


def _run_wout(inp, mods, layer, w_out, mixT, xT):
    maps = []
    for i in range(8):
        d = dict(mixT=mixT[i], w_out=w_out, xT=xT[i])
        d.update(core_vec_inputs(inp, mods, layer, i // 2))
        maps.append(d)
    r = _run(build_L2b(), maps)
    return [r[i]["xoT"] for i in range(8)]


def _run_ffn(inp, mods, layer, xT):
    maps = []
    cw = conv_w_pc(inp["ffn_conv_w"][layer])
    cb = vec_pc(inp["ffn_conv_b"][layer])
    for i in range(8):
        s = i % 2
        xh = np.ascontiguousarray(xT[i - 1][:, -2:]) if s else np.zeros((D, 2), np.float32)
        d = dict(xT=xT[i], xh=xh, flag=np.full((128, 1), float(s), np.float32), w_up=inp["ffn_w_up"][layer],
                 w_down=inp["ffn_w_down"][layer], conv_w=cw, conv_b=cb)
        d.update(core_vec_inputs(inp, mods, layer, i // 2))
        maps.append(d)
    r = _run(build_L3(), maps)
    return [r[i]["xoT"] for i in range(8)]


def kernel(**inputs):
    inp = {k: np.asarray(v) for k, v in inputs.items()}
    mods = run_L0(inp)
    x = inp["x"]
    xT = [np.ascontiguousarray(x[i // 2, (i % 2) * NTOK:(i % 2 + 1) * NTOK].T) for i in range(8)]
    maps = []
    for i in range(8):
        d = dict(xT=xT[i], w_in=inp["ev_w_in"][0])
        d.update(core_vec_inputs(inp, mods, 0, i // 2))
        maps.append(d)
    r1 = _run(build_L1(), maps)
    maps = []
    sinkbc = np.ascontiguousarray(np.broadcast_to(inp["ev_sinks"][0][None], (128, 16)).astype(np.float32))
    for i in range(8):
        s = i % 2
        prev = r1[i - 1] if s else None
        o = r1[i]
        maps.append(dict(qT=o["qT"], kTe=halo_cat(prev and prev["kT"], o["kT"], 128, 2), ve=halo_cat(prev and prev["v"], o["v"], 128, 0),
                         swa_mask=swa_consts(), flag=np.full((128, 1), float(s), np.float32), sinkbc=sinkbc, ident=_ident(),
                         pTe=halo_cat(prev and prev["pT"], o["pT"], 16, 1), invpos=invpos_table(s), pool_w=inp["ev_pool_w"][0],
                         pool_b=vec_pc(inp["ev_pool_b"][0].reshape(-1)), pool_scale=vec_pc(inp["ev_pool_scale"][0])))
    r2 = _run(build_L2a(), maps)
    xT = _run_wout(inp, mods, 0, inp["ev_w_out"][0], [r2[i]["mixT"] for i in range(8)], xT)
    xT = _run_ffn(inp, mods, 0, xT)
    maps = []
    for i in range(8):
        d = dict(xT=xT[i], w_in=inp["od_w_in"][0])
        d.update(core_vec_inputs(inp, mods, 1, i // 2))
        maps.append(d)
    r4 = _run(build_L4(), maps)
    cat = lambda key, b, ax: np.concatenate([r4[2 * b][key], r4[2 * b + 1][key]], axis=ax)
    maps_a, maps_b = [], []
    for b in range(4):
        qkT, vc, oc, gc = cat("qkT", b, 1), cat("vc", b, 0), cat("oc", b, 0), cat("gc", b, 0)
        qdT, kdT, vd = cat("qdT", b, 2), cat("kdT", b, 2), cat("vd", b, 0)
        for hh in range(2):
            maps_a.append(mlstm_inputs(inp, qkT, vc, oc, gc, hh))
            maps_b.append(moba_inputs(qdT, kdT, vd, hh))
    r5a = _run(build_L5a(), maps_a)
    r5b = _run(build_L5b(), maps_b)
    mixT = []
    for i in range(8):
        b, s = i // 2, i % 2
        sl = slice(s * NTOK, (s + 1) * NTOK)
        mixT.append(np.ascontiguousarray(np.concatenate([r5a[2 * b]["hcT"][:, sl], r5a[2 * b + 1]["hcT"][:, sl],
                                                         r5b[2 * b]["hdT"][:, sl], r5b[2 * b + 1]["hdT"][:, sl]], axis=0)))
    xT = _run_wout(inp, mods, 1, inp["od_w_out"][0], mixT, xT)
    xT = _run_ffn(inp, mods, 1, xT)
    out = np.empty((4, 2 * NTOK, D), np.float32)
    for i in range(8):
        out[i // 2, (i % 2) * NTOK:(i % 2 + 1) * NTOK] = xT[i].T
    return out
```
